# Optimizing a Trainium2 kernel written in Bass

```python
import math
import jax, jax.numpy as jnp
from jax import lax
import numpy as np

D_MODEL = 1024
BATCH = 8
SEQ = 4096
DEPTH = 2

D_MIX = D_MODEL
D_S5 = D_MIX // 2
S5_GROUP = 16
S5_GROUPS = D_S5 // S5_GROUP
S5_STATE = 64
D_GLA = D_MIX - D_S5
GLA_HEADS = 4
GLA_DV = D_GLA // GLA_HEADS
GLA_DK = GLA_DV // 2
D_GLA_K = GLA_HEADS * GLA_DK
GLA_GATE_RANK = 16
GLA_TAU = 16.0
GLA_CHUNK = 64
D_IN = D_S5 + 2 * D_GLA_K + 2 * D_GLA + GLA_GATE_RANK
SPLITS = (D_S5, D_S5 + D_GLA_K, D_S5 + 2 * D_GLA_K, D_S5 + 2 * D_GLA_K + D_GLA,
          D_S5 + 2 * D_GLA_K + 2 * D_GLA)
D_FF = 2816
N_EXPERTS = 8
TOP_K = 2
D_FF_EXPERT = 3584
N_DENSE = (DEPTH + 1) // 2
N_MOE = DEPTH // 2
EPS = 1e-6
DT_MIN = 1e-3
DT_MAX = 1e-1

kernel_name = "hymba_s5_gla_moe_trunk"


def rms_norm(x, g):
    xf = x.astype(jnp.float32)
    y = xf * lax.rsqrt(jnp.mean(xf * xf, axis=-1, keepdims=True) + EPS)
    return (y * g.astype(jnp.float32)).astype(x.dtype)


def swiglu(h, w_gate, w_up, w_down):
    return (jax.nn.silu(h @ w_gate) * (h @ w_up)) @ w_down


def s5_mixer(u, lam_re, lam_im, log_dt, b_re, b_im, c_re, c_im, d_skip, w_glu, b_glu):
    f32 = jnp.float32
    bsz, seq, _ = u.shape
    uf = u.astype(f32).reshape(bsz, seq, S5_GROUPS, S5_GROUP)
    lr = jnp.minimum(lam_re.astype(f32), -1e-4)
    li = lam_im.astype(f32)
    dt = jnp.exp(log_dt.astype(f32))[:, None]
    mag = jnp.exp(lr * dt)
    ab_re = mag * jnp.cos(li * dt)
    ab_im = mag * jnp.sin(li * dt)
    nr = ab_re - 1.0
    ni = ab_im
    den = lr * lr + li * li
    f_re = (nr * lr + ni * li) / den
    f_im = (ni * lr - nr * li) / den
    br = b_re.astype(f32)
    bi = b_im.astype(f32)
    bb_re = f_re[..., None] * br - f_im[..., None] * bi
    bb_im = f_re[..., None] * bi + f_im[..., None] * br
    bu_re = jnp.einsum('blgc,gpc->blgp', uf, bb_re)
    bu_im = jnp.einsum('blgc,gpc->blgp', uf, bb_im)
    a_re = jnp.broadcast_to(ab_re, bu_re.shape)
    a_im = jnp.broadcast_to(ab_im, bu_im.shape)

    def combine(e1, e2):
        a1r, a1i, b1r, b1i = e1
        a2r, a2i, b2r, b2i = e2
        return (a1r * a2r - a1i * a2i,
                a1r * a2i + a1i * a2r,
                a2r * b1r - a2i * b1i + b2r,
                a2r * b1i + a2i * b1r + b2i)

    _, _, h_re, h_im = lax.associative_scan(combine, (a_re, a_im, bu_re, bu_im), axis=1)
    y = (jnp.einsum('blgp,gcp->blgc', h_re, c_re.astype(f32))
         - jnp.einsum('blgp,gcp->blgc', h_im, c_im.astype(f32)))
    y = y + d_skip.astype(f32).reshape(S5_GROUPS, S5_GROUP) * uf
    y = jax.nn.gelu(y.reshape(bsz, seq, D_S5))
    y = y * jax.nn.sigmoid(y @ w_glu.astype(f32) + b_glu.astype(f32))
    return y


def gla_mixer(q, k, v, r, g_low, w_a2, b_a2, g_norm):
    f32 = jnp.float32
    bsz, seq, _ = q.shape
    n_chunks = seq // GLA_CHUNK
    c = GLA_CHUNK

    def heads(t, dh):
        return t.astype(f32).reshape(bsz, n_chunks, c, GLA_HEADS, dh).transpose(0, 3, 1, 2, 4)

    qh = heads(q, GLA_DK) * (GLA_DK ** -0.5)
    kh = heads(k, GLA_DK)
    vh = heads(v, GLA_DV)
    log_a = jax.nn.log_sigmoid(g_low.astype(f32) @ w_a2.astype(f32) + b_a2.astype(f32)) / GLA_TAU
    cum = jnp.cumsum(heads(log_a, GLA_DK), axis=3)
    cum_last = cum[..., -1:, :]
    q_t = qh * jnp.exp(cum)
    k_t = kh * jnp.exp(-cum)
    k_end = kh * jnp.exp(cum_last - cum)
    causal = jnp.tril(jnp.ones((c, c), dtype=bool))
    scores = jnp.where(causal, jnp.einsum('bhnik,bhnjk->bhnij', q_t, k_t), 0.0)
    o_intra = jnp.einsum('bhnij,bhnjv->bhniv', scores, vh)
    upd = jnp.einsum('bhnjk,bhnjv->bhnkv', k_end, vh)
    decay = jnp.exp(cum_last[..., 0, :])

    def step(state, inp):
        dec, u = inp
        return dec[..., None] * state + u, state

    s0 = jnp.zeros((bsz, GLA_HEADS, GLA_DK, GLA_DV), f32)
    _, s_prev = lax.scan(step, s0, (jnp.moveaxis(decay, 2, 0), jnp.moveaxis(upd, 2, 0)))
    s_prev = jnp.moveaxis(s_prev, 0, 2)
    o = o_intra + jnp.einsum('bhnik,bhnkv->bhniv', q_t, s_prev)
    o = o * lax.rsqrt(jnp.mean(o * o, axis=-1, keepdims=True) + EPS)
    o = o.transpose(0, 2, 3, 1, 4).reshape(bsz, seq, D_GLA) * g_norm.astype(f32)
    return o * jax.nn.silu(r.astype(f32))


def moe_swiglu(h, w_router, w_gate, w_up, w_down):
    f32 = jnp.float32
    logits = h.astype(f32) @ w_router.astype(f32)
    top_v, top_i = lax.top_k(logits, TOP_K)
    wts = jax.nn.softmax(top_v, axis=-1)
    gates = jnp.sum(jax.nn.one_hot(top_i, N_EXPERTS, dtype=f32) * wts[..., None], axis=-2)
    out = jnp.zeros(h.shape, f32)
    for e in range(N_EXPERTS):
        out = out + gates[..., e:e + 1] * swiglu(h, w_gate[e], w_up[e], w_down[e]).astype(f32)
    return out


def setup_inputs(seed: int = 0) -> dict:
    key = jax.random.key(seed)
    ks = jax.random.split(key, 32)
    f32 = jnp.float32

    def nrm(k, shape, scale):
        return jax.random.normal(k, shape, f32) * scale

    n_idx = jnp.arange(S5_STATE, dtype=f32)
    return {
        "x": nrm(ks[0], (BATCH, SEQ, D_MODEL), 1.0),
        "norm_mix": 1.0 + nrm(ks[1], (DEPTH, D_MODEL), 0.02),
        "w_in": nrm(ks[2], (DEPTH, D_MODEL, D_IN), D_MODEL ** -0.5),
        "s5_lambda_re": -0.5 + nrm(ks[3], (DEPTH, S5_GROUPS, S5_STATE), 0.01),
        "s5_lambda_im": math.pi * n_idx + nrm(ks[4], (DEPTH, S5_GROUPS, S5_STATE), 0.01),
        "s5_log_dt": jax.random.uniform(ks[5], (DEPTH, S5_GROUPS), f32,
                                        minval=math.log(DT_MIN), maxval=math.log(DT_MAX)),
        "s5_b_re": nrm(ks[6], (DEPTH, S5_GROUPS, S5_STATE, S5_GROUP), (2 * S5_GROUP) ** -0.5),
        "s5_b_im": nrm(ks[7], (DEPTH, S5_GROUPS, S5_STATE, S5_GROUP), (2 * S5_GROUP) ** -0.5),
        "s5_c_re": nrm(ks[8], (DEPTH, S5_GROUPS, S5_GROUP, S5_STATE), S5_STATE ** -0.5),
        "s5_c_im": nrm(ks[9], (DEPTH, S5_GROUPS, S5_GROUP, S5_STATE), S5_STATE ** -0.5),
        "s5_d": nrm(ks[10], (DEPTH, D_S5), 1.0),
        "s5_w_glu": nrm(ks[11], (DEPTH, D_S5, D_S5), D_S5 ** -0.5),
        "s5_b_glu": nrm(ks[12], (DEPTH, D_S5), 0.02),
        "s5_out_norm": 1.0 + nrm(ks[13], (DEPTH, D_S5), 0.02),
        "gla_w_a2": nrm(ks[14], (DEPTH, GLA_GATE_RANK, D_GLA_K), GLA_GATE_RANK ** -0.5),
        "gla_b_a2": nrm(ks[15], (DEPTH, D_GLA_K), 0.1),
        "gla_out_norm": 1.0 + nrm(ks[16], (DEPTH, D_GLA), 0.02),
        "w_out": nrm(ks[17], (DEPTH, D_MIX, D_MODEL), D_MIX ** -0.5),
        "norm_ffn": 1.0 + nrm(ks[18], (DEPTH, D_MODEL), 0.02),
        "ffn_w_gate": nrm(ks[19], (N_DENSE, D_MODEL, D_FF), D_MODEL ** -0.5),
        "ffn_w_up": nrm(ks[20], (N_DENSE, D_MODEL, D_FF), D_MODEL ** -0.5),
        "ffn_w_down": nrm(ks[21], (N_DENSE, D_FF, D_MODEL), D_FF ** -0.5),
        "moe_w_router": nrm(ks[22], (N_MOE, D_MODEL, N_EXPERTS), D_MODEL ** -0.5),
        "moe_w_gate": nrm(ks[23], (N_MOE, N_EXPERTS, D_MODEL, D_FF_EXPERT), D_MODEL ** -0.5),
        "moe_w_up": nrm(ks[24], (N_MOE, N_EXPERTS, D_MODEL, D_FF_EXPERT), D_MODEL ** -0.5),
        "moe_w_down": nrm(ks[25], (N_MOE, N_EXPERTS, D_FF_EXPERT, D_MODEL), D_FF_EXPERT ** -0.5),
        "norm_final": 1.0 + nrm(ks[26], (D_MODEL,), 0.02),
    }


def reference(x, norm_mix, w_in, s5_lambda_re, s5_lambda_im, s5_log_dt, s5_b_re, s5_b_im,
              s5_c_re, s5_c_im, s5_d, s5_w_glu, s5_b_glu, s5_out_norm, gla_w_a2, gla_b_a2,
              gla_out_norm, w_out, norm_ffn, ffn_w_gate, ffn_w_up, ffn_w_down, moe_w_router,
              moe_w_gate, moe_w_up, moe_w_down, norm_final):
    for l in range(DEPTH):
        h = rms_norm(x, norm_mix[l])
        proj = h @ w_in[l]
        u, q, k, v, r, g_low = jnp.split(proj, SPLITS, axis=-1)
        y_s5 = s5_mixer(u, s5_lambda_re[l], s5_lambda_im[l], s5_log_dt[l], s5_b_re[l],
                        s5_b_im[l], s5_c_re[l], s5_c_im[l], s5_d[l], s5_w_glu[l], s5_b_glu[l])
        y_s5 = rms_norm(y_s5, s5_out_norm[l])
        y_gla = gla_mixer(q, k, v, r, g_low, gla_w_a2[l], gla_b_a2[l], gla_out_norm[l])
        mixed = jnp.concatenate([y_s5, y_gla], axis=-1).astype(x.dtype)
        x = x + mixed @ w_out[l]
        h = rms_norm(x, norm_ffn[l])
        i = l // 2
        if l % 2 == 0:
            x = x + swiglu(h, ffn_w_gate[i], ffn_w_up[i], ffn_w_down[i]).astype(x.dtype)
        else:
            x = x + moe_swiglu(h, moe_w_router[i], moe_w_gate[i], moe_w_up[i],
                               moe_w_down[i]).astype(x.dtype)
    return rms_norm(x, norm_final)
```

```python
import contextlib
import math
import numpy as np
import concourse.bass as bass
import concourse.mybir as mybir
from concourse.bass_utils import run_bass_kernel_spmd

F32 = mybir.dt.float32
BF16 = mybir.dt.bfloat16
I32 = mybir.dt.int32
AF = mybir.ActivationFunctionType
ALU = mybir.AluOpType

D = 1024
D_IN = 2064
D_FF = 2816
D_FFE = 3584
NE = 8
EPS = 1e-6
TWO_PI = 2.0 * math.pi

COMPUTE = ("pe", "act", "dve", "pool")
NO_SELF_WAIT = ()


class Tok:
    __slots__ = ("name", "w", "r")

    def __init__(self, name):
        self.name = name
        self.w = None
        self.r = {}


class Prog:
    def __init__(self, nc):
        self.nc = nc
        self.ops = {e: [] for e in ("pe", "act", "dve", "pool", "sync")}
        self.seq = {e: 0 for e in COMPUTE}
        self.waited = {e: {} for e in self.ops}
        self.dma_cnt = {}
        self.semkeys = []
        self.pending_sig = {e: False for e in COMPUTE}

    def _semkey(self, k):
        if k not in self.semkeys:
            self.semkeys.append(k)
        return k

    def _deps(self, reads, writes):
        deps = {}

        def add(k, v):
            if deps.get(k, 0) < v:
                deps[k] = v
        for t in reads:
            if t.w is not None:
                add(*t.w)
        for t in writes:
            if t.w is not None:
                add(*t.w)
            for k, v in t.r.items():
                add(k, v)
        return deps

    def _emit_waits(self, eng, deps):
        for k, v in deps.items():
            if k == eng and (eng == "pe" or eng in NO_SELF_WAIT):
                continue
            if self.waited[eng].get(k, 0) >= v:
                continue
            self.waited[eng][k] = v
            self.ops[eng].append(("wait", k, v))

    def op(self, eng, fn, reads=(), writes=(), sig=True):
        deps = self._deps(reads, writes)
        self._emit_waits(eng, deps)
        if sig:
            self.seq[eng] += 1
            comp = (eng, self.seq[eng])
            self.pending_sig[eng] = False
        else:
            comp = (eng, self.seq[eng] + 1)
            self.pending_sig[eng] = True
        self._semkey(eng)
        self.ops[eng].append(("op", fn, sig, eng))
        for t in writes:
            t.w = comp
            t.r = {}
        for t in reads:
            if t.r.get(comp[0], 0) < comp[1]:
                t.r[comp[0]] = comp[1]

    def frontier(self):
        f = {e: self.seq[e] + (1 if self.pending_sig[e] else 0) for e in COMPUTE if self.seq[e] or self.pending_sig[e]}
        f.update(self.dma_cnt)
        return f

    def dma(self, q, fn, key, reads=(), writes=()):
        deps = self._deps(reads, writes)
        k = self._semkey("dma_" + key)
        if self.dma_cnt.get(k, 0) > 0 and deps.get(k, 0) < self.dma_cnt[k]:
            deps[k] = self.dma_cnt[k]
        self._emit_waits(q, deps)
        self.dma_cnt[k] = self.dma_cnt.get(k, 0) + 16
        comp = (k, self.dma_cnt[k])
        self.ops[q].append(("dma", fn, k))
        for t in writes:
            t.w = comp
            t.r = {}
        for t in reads:
            if t.r.get(k, 0) < comp[1]:
                t.r[k] = comp[1]
        return comp

    def finish(self, final_waits):
        nc = self.nc
        for e in COMPUTE:
            assert not self.pending_sig[e], f"pending unsignalled op on {e}"
        with contextlib.ExitStack() as st:
            sems = {}
            for k in self.semkeys:
                sems[k] = st.enter_context(nc.semaphore(k))
            block = st.enter_context(nc.Block())

            def replay(ename, eng_obj, extra=()):
                for it in self.ops[ename]:
                    if it[0] == "wait":
                        eng_obj.wait_ge(sems[it[1]], it[2])
                    elif it[0] == "op":
                        ins = it[1](eng_obj)
                        if it[2]:
                            ins.then_inc(sems[it[3]], 1)
                    else:
                        ins = it[1](eng_obj)
                        ins.then_inc(sems[it[2]], 16)
                for k, v in extra:
                    eng_obj.wait_ge(sems[k], v)

            @block.tensor
            def _(e):
                replay("pe", e)

            @block.scalar
            def _(e):
                replay("act", e)

            @block.vector
            def _(e):
                replay("dve", e)

            @block.gpsimd
            def _(e):
                replay("pool", e, extra=final_waits)

            @block.sync
            def _(e):
                replay("sync", e)


class K:
    def __init__(self, L, stop_after=None, cut=99):
        self.L = L
        self.cut = cut
        self.stop_after = stop_after
        self.nc = bass.Bass("TRN2", target_bir_lowering=False)
        self.P = Prog(self.nc)
        self.toks = {}
        self.base = {}

    def tok(self, name):
        if name not in self.toks:
            t = Tok(name)
            t.r = dict(self.base)
            self.toks[name] = t
        return self.toks[name]

    def fence(self):
        self.base = self.P.frontier()

    def pst(self, i):
        return [f"ps{i}"]

    def mm(self, out, lhsT, rhs, start, stop, r, w, sig=True):
        self.P.op("pe", lambda e: e.matmul(out, lhsT, rhs, start=start, stop=stop),
                  reads=[self.tok(x) for x in r], writes=[self.tok(x) for x in w], sig=sig)

    def tr(self, out, in_, ident, r, w, sig=True):
        self.P.op("pe", lambda e: e.transpose(out, in_, ident),
                  reads=[self.tok(x) for x in r], writes=[self.tok(x) for x in w], sig=sig)

    def act(self, out, in_, func, r, w, bias=None, scale=None, accum=None):
        kw = {}
        if bias is not None:
            kw["bias"] = bias
        if scale is not None:
            kw["scale"] = scale
        if accum is not None:
            kw["accum_out"] = accum
        self.P.op("act", lambda e: e.activation(out=out, in_=in_, func=func, **kw),
                  reads=[self.tok(x) for x in r], writes=[self.tok(x) for x in w])

    def ts(self, out, in0, s1, s2, op0, op1, r, w, eng="dve"):
        if op1 is None:
            f = lambda e: e.tensor_scalar(out=out, in0=in0, scalar1=s1, scalar2=None, op0=op0)
        else:
            f = lambda e: e.tensor_scalar(out=out, in0=in0, scalar1=s1, scalar2=s2, op0=op0, op1=op1)
        self.P.op(eng, f, reads=[self.tok(x) for x in r], writes=[self.tok(x) for x in w])

    def stt(self, out, in0, scalar, in1, op0, op1, r, w):
        self.P.op("dve", lambda e: e.scalar_tensor_tensor(out=out, in0=in0, scalar=scalar, in1=in1,
                                                          op0=op0, op1=op1),
                  reads=[self.tok(x) for x in r], writes=[self.tok(x) for x in w])

    def tt(self, out, in0, in1, op, r, w, eng="dve"):
        self.P.op(eng, lambda e: e.tensor_tensor(out=out, in0=in0, in1=in1, op=op),
                  reads=[self.tok(x) for x in r], writes=[self.tok(x) for x in w])

    def cp(self, out, in_, r, w, eng="dve"):
        if eng == "act":
            return self.act(out, in_, AF.Copy, r, w)
        if eng == "dve" and any(x.startswith("ps") for x in r):
            return self.ts(out, in_, 1.0, None, ALU.mult, None, r, w)
        self.P.op(eng, lambda e: e.tensor_copy(out=out, in_=in_),
                  reads=[self.tok(x) for x in r], writes=[self.tok(x) for x in w])

    def memset(self, ap, val, w, eng="pool"):
        self.P.op(eng, lambda e: e.memset(ap, val), writes=[self.tok(x) for x in w])

    def dma(self, q, out, in_, key, r, w, **kw):
        return self.P.dma(q, lambda e: e.dma_start(out=out, in_=in_, **kw), key,
                          reads=[self.tok(x) for x in r], writes=[self.tok(x) for x in w])

    def rsqrt_(self, out, in_, tmp, r, w, eps=EPS, mul=1.0):
        self.ts(tmp, in_, mul, eps, ALU.mult, ALU.add, r, w)
        self.act(tmp, tmp, AF.Sqrt, w, w)
        self.P.op("dve", lambda e: e.reciprocal(out=out, in_=tmp),
                  reads=[self.tok(x) for x in w], writes=[self.tok(x) for x in w])

    def build(self):
        nc = self.nc
        L = self.L
        dr = {}

        def din(name, shape):
            dr[name] = nc.dram_tensor(name, list(shape), F32, kind="ExternalInput").ap()
        din("x", [L, D])
        din("norm_mix", [2, D]); din("w_in", [2, D, D_IN])
        din("s5_lambda_re", [2, 32, 64]); din("s5_lambda_im", [2, 32, 64]); din("s5_log_dt", [2, 32])
        din("s5_b_re", [2, 32, 64, 16]); din("s5_b_im", [2, 32, 64, 16])
        din("s5_c_re", [2, 32, 16, 64]); din("s5_c_im", [2, 32, 16, 64])
        din("s5_d", [2, 512]); din("s5_w_glu", [2, 512, 512]); din("s5_b_glu", [2, 512])
        din("s5_out_norm", [2, 512]); din("gla_w_a2", [2, 16, 256]); din("gla_b_a2", [2, 256])
        din("gla_out_norm", [2, 512]); din("w_out", [2, D, D]); din("norm_ffn", [2, D])
        din("ffn_w_gate", [1, D, D_FF]); din("ffn_w_up", [1, D, D_FF]); din("ffn_w_down", [1, D_FF, D])
        din("moe_w_router", [1, D, NE]); din("moe_w_gate", [1, NE, D, D_FFE])
        din("moe_w_up", [1, NE, D, D_FFE]); din("moe_w_down", [1, NE, D_FFE, D])
        din("norm_final", [D])
        self.dr = dr
        y = nc.dram_tensor("y", [L, D], F32, kind="ExternalOutput").ap()
        s1 = nc.dram_tensor("scr1", [L, D], F32, kind="Internal").ap()
        s2 = nc.dram_tensor("scr2", [L, D], F32, kind="Internal").ap()
        sa = self.stop_after
        with contextlib.ExitStack() as st:
            self.st = st
            self.psum = [st.enter_context(nc.psum_tensor(f"ps{i}", [128, 512], F32)) for i in range(8)]
            self.consts()
            if sa == "mix0":
                self.mixer(0, dr["x"], y)
            elif sa == "ffn0":
                self.mixer(0, dr["x"], s1)
                self.ffn(0, s1, y, final=False)
            elif sa == "mix1":
                self.mixer(0, dr["x"], s1)
                self.ffn(0, s1, s2, final=False)
                self.mixer(1, s2, y)
            elif sa == "ffnonly":
                self.ffn(0, dr["x"], y, final=False)
            elif sa == "moeonly":
                self.ffn(1, dr["x"], y, final=True)
            else:
                self.mixer(0, dr["x"], s1)
                self.ffn(0, s1, s2, final=False)
                self.mixer(1, s2, s1)
                self.ffn(1, s1, y, final=True)
            fw = [(k, v) for k, v in self.P.dma_cnt.items() if k.startswith("dma_yout")]
            self.P.finish(fw)
        return nc

    def sb(self, stack, name, shape, dt=F32):
        return stack.enter_context(self.nc.sbuf_tensor(name, list(shape), dt))

    def consts(self):
        st = self.st
        self.ident = self.sb(st, "ident", [128, 128])
        self.onesf = self.sb(st, "onesf", [128, 128])
        self.memset(self.onesf[:], 1.0, ["onesf"])
        self.P.op("pool", lambda e: e.affine_select(out=self.ident[:], in_=self.onesf[:], pattern=[[-1, 128]],
                                                    compare_op=ALU.is_equal, fill=0.0, base=0,
                                                    channel_multiplier=1),
                  reads=[self.tok("onesf")], writes=[self.tok("ident")])
        self.triM = self.sb(st, "triM", [128, 128])
        self.triU = self.sb(st, "triU", [128, 128])
        self.maskC = self.sb(st, "maskC", [128, 128])
        self.blk = self.sb(st, "blkm", [128, 128])
        self.memset(self.blk[:], 0.0, ["blkm"])
        self.memset(self.blk[0:64, 0:64], 1.0, ["blkm"])
        self.memset(self.blk[64:128, 64:128], 1.0, ["blkm"])
        self.P.op("pool", lambda e: e.affine_select(out=self.maskC[:], in_=self.blk[:], pattern=[[1, 128]],
                                                    compare_op=ALU.is_ge, fill=0.0, base=0,
                                                    channel_multiplier=-1),
                  reads=[self.tok("blkm")], writes=[self.tok("maskC")])
        self.ts(self.triM[:], self.maskC[:], -1.0 / 16.0, None, ALU.mult, None, ["maskC"], ["triM"], eng="pool")
        self.P.op("pool", lambda e: e.affine_select(out=self.triU[:], in_=self.blk[:], pattern=[[-1, 128]],
                                                    compare_op=ALU.is_gt, fill=0.0, base=0,
                                                    channel_multiplier=1),
                  reads=[self.tok("blkm")], writes=[self.tok("triU")])
        self.ts(self.triU[:], self.triU[:], -1.0 / 16.0, None, ALU.mult, None, ["triU"], ["triU"], eng="pool")
        self.avg512 = self.sb(st, "avg512", [128, 128], BF16)
        self.avg128 = self.sb(st, "avg128", [128, 128], BF16)
        self.memset(self.avg512[:], 1.0 / 512.0, ["avg512"])
        self.memset(self.avg128[:], 1.0 / 128.0, ["avg128"])
        self.ones1 = self.sb(st, "ones1", [1, 128])
        self.memset(self.ones1[:], 1.0, ["ones1"])

    def norm_block(self, stk, pfx, xs_ap, xtok, gT, gtok, hT_dst, hT_tok, ps2, pstoks, scr, h32_dst=None):
        stt_ = scr["stat"]
        self.act(scr["junk"][:], xs_ap, AF.Square, [xtok], [pfx + "junk", pfx + "stat"], accum=stt_[:, 0:1])
        self.rsqrt_(stt_[:, 1:2], stt_[:, 0:1], stt_[:, 2:3], [pfx + "stat"], [pfx + "stat"], mul=1.0 / D)
        self.ts(scr["xn"][:], xs_ap, stt_[:, 1:2], None, ALU.mult, None, [xtok, pfx + "stat"], [pfx + "xn"])
        for k in range(8):
            self.tr(ps2[k // 4][:, (k % 4) * 128:(k % 4 + 1) * 128], scr["xn"][:, k * 128:(k + 1) * 128],
                    self.ident[:], [pfx + "xn", "ident"], [pstoks[k // 4]], sig=(k % 4 == 3))
        for hb in range(2):
            self.tt(hT_dst[:, hb * 4:(hb + 1) * 4, :],
                    ps2[hb][:, :].rearrange("p (k t) -> p k t", k=4),
                    gT[:, hb * 4:(hb + 1) * 4].unsqueeze(2).to_broadcast([128, 4, 128]),
                    ALU.mult, [pstoks[hb], gtok], [hT_tok])
            if h32_dst is not None:
                self.tt(h32_dst[:, hb * 4:(hb + 1) * 4, :],
                        ps2[hb][:, :].rearrange("p (k t) -> p k t", k=4),
                        gT[:, hb * 4:(hb + 1) * 4].unsqueeze(2).to_broadcast([128, 4, 128]),
                        ALU.mult, [pstoks[hb], gtok], [hT_tok + "32"])

    def load_colvec(self, dst, src_flat, key, tokn):
        self.dma("sync", dst, src_flat.rearrange("(k q) -> q k", q=128), key, [], [tokn],
                 allow_slow_non_contiguous=True)

    def mixer(self, l, xin, xout):
        nc, dr, L = self.nc, self.dr, self.L
        T = 256
        NT = L // T
        pfx = f"m{l}_"
        PS = self.psum
        self.fence()
        with contextlib.ExitStack() as sk:
            sb = lambda name, shape, dt=F32: self.sb(sk, pfx + name, shape, dt)
            Win = sb("Win", [128, 8, D_IN], BF16)
            for c in range(3):
                self.dma("pool", Win[:, :, c * 688:(c + 1) * 688],
                         dr["w_in"][l].rearrange("(k q) n -> q k n", q=128)[:, :, c * 688:(c + 1) * 688],
                         pfx + f"Win{c}", [], [pfx + f"Win{c}"])
            WIN = [pfx + f"Win{c}" for c in range(3)]
            Wout = sb("Wout", [128, 8, D], BF16)
            self.dma("pool", Wout[:], dr["w_out"][l].rearrange("(k q) n -> q k n", q=128), pfx + "Wout", [], [pfx + "Wout"])
            Wglu = sb("Wglu", [128, 4, 512], BF16)
            self.dma("pool", Wglu[:], dr["s5_w_glu"][l].rearrange("(k q) n -> q k n", q=128), pfx + "Wglu", [], [pfx + "Wglu"])
            Wa2 = sb("Wa2", [16, 256])
            self.dma("sync", Wa2[:], dr["gla_w_a2"][l], pfx + "Wa2", [], [pfx + "Wa2"])
            ba2 = sb("ba2", [1, 256])
            self.dma("sync", ba2[:], dr["gla_b_a2"][l:l + 1, :], pfx + "ba2", [], [pfx + "ba2"])
            gmix = sb("gmix", [128, 8]); self.load_colvec(gmix[:], dr["norm_mix"][l], pfx + "gmix", pfx + "gmix")
            dsk = sb("dsk", [128, 4]); self.load_colvec(dsk[:], dr["s5_d"][l], pfx + "dsk", pfx + "dsk")
            bglu = sb("bglu", [128, 4]); self.load_colvec(bglu[:], dr["s5_b_glu"][l], pfx + "bglu", pfx + "bglu")
            gs5 = sb("gs5", [128, 4]); self.load_colvec(gs5[:], dr["s5_out_norm"][l], pfx + "gs5", pfx + "gs5")
            ggla = sb("ggla", [128, 4]); self.load_colvec(ggla[:], dr["gla_out_norm"][l], pfx + "ggla", pfx + "ggla")
            prm = sb("prm", [128, 24, 16])
            PT = pfx + "prm"

            def pr(i):
                return prm[:, i, :]
            LR, LI, DT, MAG, TH, N_, SN, CS, ABR, ABI, DEN, FRE, FIM, T1, T2, T3 = range(16)
            self.dma("sync", pr(LR), dr["s5_lambda_re"][l].rearrange("(k two) p -> (two p) k", two=2), pfx + "lr", [], [PT],
                     allow_slow_non_contiguous=True)
            self.dma("sync", pr(LI), dr["s5_lambda_im"][l].rearrange("(k two) p -> (two p) k", two=2), pfx + "li", [], [PT],
                     allow_slow_non_contiguous=True)
            for two in range(2):
                self.dma("sync", prm[two * 64:(two + 1) * 64, DT, :],
                         dr["s5_log_dt"][l].rearrange("(k two) -> two k", two=2)[two].partition_broadcast(64),
                         pfx + f"dt{two}", [], [PT], allow_slow_non_contiguous=True)
            self.ts(pr(LR), pr(LR), -1e-4, None, ALU.min, None, [PT], [PT])
            self.act(pr(DT), pr(DT), AF.Exp, [PT], [PT])
            self.tt(pr(T1), pr(LR), pr(DT), ALU.mult, [PT], [PT])
            self.act(pr(MAG), pr(T1), AF.Exp, [PT], [PT])
            self.tt(pr(TH), pr(LI), pr(DT), ALU.mult, [PT], [PT])
            ni = sb("ni", [128, 16], I32)

            def reduce_angle(ap, shape_ni, tmpf, toks):
                self.ts(tmpf, ap, 1.0 / TWO_PI, None, ALU.mult, None, toks, toks)
                self.cp(shape_ni, tmpf, toks, toks)
                self.cp(tmpf, shape_ni, toks, toks)
                self.stt(ap, tmpf, -TWO_PI, ap, ALU.mult, ALU.add, toks, toks)
                self.ts(tmpf, ap, math.pi, -TWO_PI, ALU.is_gt, ALU.mult, toks, toks)
                self.tt(ap, ap, tmpf, ALU.add, toks, toks)
                self.ts(tmpf, ap, -math.pi, TWO_PI, ALU.is_lt, ALU.mult, toks, toks)
                self.tt(ap, ap, tmpf, ALU.add, toks, toks)
            reduce_angle(pr(TH), ni[:], pr(T1), [PT])
            self.act(pr(SN), pr(TH), AF.Sin, [PT], [PT])
            self.ts(pr(T2), pr(TH), math.pi / 2, None, ALU.add, None, [PT], [PT])
            reduce_angle(pr(T2), ni[:], pr(T1), [PT])
            self.act(pr(CS), pr(T2), AF.Sin, [PT], [PT])
            self.tt(pr(ABR), pr(MAG), pr(CS), ALU.mult, [PT], [PT])
            self.tt(pr(ABI), pr(MAG), pr(SN), ALU.mult, [PT], [PT])
            self.ts(pr(ABR), pr(ABR), -1.0, None, ALU.add, None, [PT], [PT])
            self.tt(pr(DEN), pr(LR), pr(LR), ALU.mult, [PT], [PT])
            self.tt(pr(T1), pr(LI), pr(LI), ALU.mult, [PT], [PT])
            self.tt(pr(DEN), pr(DEN), pr(T1), ALU.add, [PT], [PT])
            self.P.op("dve", lambda e: e.reciprocal(out=pr(DEN), in_=pr(DEN)), reads=[self.tok(PT)], writes=[self.tok(PT)])
            self.tt(pr(T1), pr(ABR), pr(LR), ALU.mult, [PT], [PT])
            self.tt(pr(T2), pr(ABI), pr(LI), ALU.mult, [PT], [PT])
            self.tt(pr(T1), pr(T1), pr(T2), ALU.add, [PT], [PT])
            self.tt(pr(FRE), pr(T1), pr(DEN), ALU.mult, [PT], [PT])
            self.tt(pr(T1), pr(ABI), pr(LR), ALU.mult, [PT], [PT])
            self.tt(pr(T2), pr(ABR), pr(LI), ALU.mult, [PT], [PT])
            self.tt(pr(T1), pr(T1), pr(T2), ALU.subtract, [PT], [PT])
            self.tt(pr(FIM), pr(T1), pr(DEN), ALU.mult, [PT], [PT])
            cosT = sb("cosT", [128, 16, T])
            sinT = sb("sinT", [128, 16, T])
            TB = pfx + "tab"
            with contextlib.ExitStack() as s2:
              if self.cut >= 2:
                  iot = self.sb(s2, pfx + "iot", [128, T])
                  self.P.op("pool", lambda e: e.iota(iot[:], [[1, T]], base=1, channel_multiplier=0,
                                                     allow_small_or_imprecise_dtypes=True),
                            writes=[self.tok(pfx + "iot")])
                  tmpA = self.sb(s2, pfx + "tmpA", [128, 16, T])
                  tmpN = self.sb(s2, pfx + "tmpN", [128, 16, T], I32)
                  ang = self.sb(s2, pfx + "ang", [128, 16, T])
                  self.tt(ang[:], prm[:, TH, :].unsqueeze(2).to_broadcast([128, 16, T]),
                          iot[:].unsqueeze(1).to_broadcast([128, 16, T]), ALU.mult, [PT, pfx + "iot"], [TB])
                  fl = lambda a: a[:].rearrange("p k t -> p (k t)")
                  reduce_angle(fl(ang), fl(tmpN), fl(tmpA), [TB])
                  self.act(fl(sinT), fl(ang), AF.Sin, [TB], [TB])
                  self.ts(fl(ang), fl(ang), math.pi / 2, None, ALU.add, None, [TB], [TB])
                  reduce_angle(fl(ang), fl(tmpN), fl(tmpA), [TB])
                  self.act(fl(cosT), fl(ang), AF.Sin, [TB], [TB])
            self.fence()
            BT = [sb("BTre", [128, 16, 128], BF16), sb("BTim", [128, 16, 128], BF16)]
            CT = [sb("CTre", [128, 16, 128], BF16), sb("CTnre", [128, 16, 128], BF16), sb("CTnim", [128, 16, 128], BF16)]
            with contextlib.ExitStack() as s2:
                bp = [self.sb(s2, pfx + f"bp{i}", [128, 16, 128]) for i in range(2)]
                bb = [self.sb(s2, pfx + f"bb{i}", [128, 16, 128]) for i in range(2)]
                cpad = [self.sb(s2, pfx + f"cp{i}", [128, 16, 128]) for i in range(2)]
                tmpB = self.sb(s2, pfx + "tmpB", [128, 16, 128])
                for i, nm in enumerate(("s5_b_re", "s5_b_im")):
                    self.memset(bp[i][:], 0.0, [pfx + f"bp{i}"])
                    for two in range(2):
                        for m in range(4):
                            dst = bp[i][two * 64:(two + 1) * 64, :, :].rearrange("p (j m) c -> p j m c", m=4)[
                                :, :, m, m * 32 + two * 16: m * 32 + two * 16 + 16]
                            src = dr[nm][l].rearrange("(j m two) p c -> two m p j c", m=4, two=2)[two, m]
                            self.dma("sync", dst, src, pfx + f"bp{i}", [], [pfx + f"bp{i}"])
                for i, nm in enumerate(("s5_c_re", "s5_c_im")):
                    self.memset(cpad[i][:], 0.0, [pfx + f"cp{i}"])
                    for two in range(2):
                        for m in range(4):
                            r0 = m * 32 + two * 16
                            dst = cpad[i][r0:r0 + 16, :, :].rearrange("p (j m) c -> p j m c", m=4)[
                                :, :, m, two * 64:(two + 1) * 64]
                            src = dr[nm][l].rearrange("(j m two) c p -> two m c j p", m=4, two=2)[two, m]
                            self.dma("sync", dst, src, pfx + f"cp{i}", [], [pfx + f"cp{i}"])
                fre_b = prm[:, FRE, :].unsqueeze(2).to_broadcast([128, 16, 128])
                fim_b = prm[:, FIM, :].unsqueeze(2).to_broadcast([128, 16, 128])
                B0, B1 = pfx + "bp0", pfx + "bp1"
                self.tt(bb[0][:], bp[0][:], fre_b, ALU.mult, [B0, PT], [pfx + "bb0"])
                self.tt(tmpB[:], bp[1][:], fim_b, ALU.mult, [B1, PT], [pfx + "tmpB"])
                self.tt(bb[0][:], bb[0][:], tmpB[:], ALU.subtract, [pfx + "bb0", pfx + "tmpB"], [pfx + "bb0"])
                self.tt(bb[1][:], bp[1][:], fre_b, ALU.mult, [B1, PT], [pfx + "bb1"])
                self.tt(tmpB[:], bp[0][:], fim_b, ALU.mult, [B0, PT], [pfx + "tmpB"])
                self.tt(bb[1][:], bb[1][:], tmpB[:], ALU.add, [pfx + "bb1", pfx + "tmpB"], [pfx + "bb1"])
                cnt = 0
                for i in range(2 if self.cut >= 3 else 0):
                    for k in range(16):
                        pb = PS[cnt % 2]; ptk = f"ps{cnt % 2}"
                        cnt += 1
                        self.tr(pb[:, 0:128], bb[i][:, k, :], self.ident[:], [pfx + f"bb{i}", "ident"], [ptk])
                        self.cp(BT[i][:, k, :], pb[:, 0:128], [ptk], [pfx + "BT"], eng="act" if k % 2 else "dve")
                for i in range(2 if self.cut >= 3 else 0):
                    for k in range(16):
                        pb = PS[cnt % 2]; ptk = f"ps{cnt % 2}"
                        cnt += 1
                        self.tr(pb[:, 0:128], cpad[i][:, k, :], self.ident[:], [pfx + f"cp{i}", "ident"], [ptk])
                        if i == 0:
                            self.cp(CT[0][:, k, :], pb[:, 0:128], [ptk], [pfx + "CT"], eng="act")
                            self.ts(CT[1][:, k, :], pb[:, 0:128], -1.0, None, ALU.mult, None, [ptk], [pfx + "CT"])
                        else:
                            self.ts(CT[2][:, k, :], pb[:, 0:128], -1.0, None, ALU.mult, None, [ptk], [pfx + "CT"])
            self.fence()
            sre = sb("sre", [128, 16]); sim = sb("sim", [128, 16])
            lre = sb("lre", [128, 16]); lim = sb("lim", [128, 16])
            ctmp = sb("ctmp", [128, 4, 16])
            self.memset(sre[:], 0.0, [pfx + "s5st"]); self.memset(sim[:], 0.0, [pfx + "s5st"])
            Sg = sb("Sg", [128, 2, 128])
            Sgbz = [[sb(f"Sgbz{r}{i}", [128, 2, 128], BF16) for i in range(2)] for r in range(2)]
            self.memset(Sg[:], 0.0, [pfx + "Sg"])
            for r in range(2):
                for i in range(2):
                    self.memset(Sgbz[r][i][:], 0.0, [pfx + f"Sgb{r}"])
            xs = [sb(f"xs{i}", [128, D]) for i in range(4)]
            scr = {"stat": sb("stat", [128, 4]), "junk": sb("junk", [128, D], BF16), "xn": sb("xn", [128, D])}
            hT = sb("hT", [128, 8, T], BF16)
            uT = sb("uT", [128, 4, T]); uTb = sb("uTb", [128, 4, T], BF16)
            qkT = sb("qkT", [128, 4, T])
            srT = sb("srT", [128, 4, T])
            glT = sb("glT", [16, T])
            vtm = sb("vtm", [128, 2, 512], BF16)
            ktm = sb("ktm", [128, 2, 256])
            kendz = [sb(f"kendz{i}", [128, 2, 256], BF16) for i in range(2)]
            qtT = sb("qtT", [128, 2, T], BF16)
            ktTz = [sb(f"ktTz{i}", [128, 2, T], BF16) for i in range(2)]
            for i in range(2):
                self.memset(kendz[i][:], 0.0, [pfx + "kend"])
                self.memset(ktTz[i][:], 0.0, [pfx + "ktT"])
            dec = sb("dec", [128, 2, 4])
            A = [[sb(f"A{q}{i}", [128, T]) for i in range(4)] for q in range(2)]
            vv = [[sb(f"vv{q}{i}", [128, T]) for i in range(2)] for q in range(2)]
            ss = [[sb(f"ss{q}{i}", [128, T]) for i in range(2)] for q in range(2)]
            pp = [[sb(f"pp{q}{i}", [128, T], BF16) for i in range(4)] for q in range(2)]
            yj = sb("yj", [128, T]); g1 = sb("g1", [128, T]); g2 = sb("g2", [128, T])
            yg = sb("yg", [128, 4, T]); ygb = sb("ygb", [128, 4, T], BF16)
            y2 = sb("y2", [128, 4, T]); sq = sb("sq", [128, 4, T], BF16)
            scTh = sb("scTh", [128, 4, 128], BF16)
            ltm = y2[:, 0:2, :]
            mixT = sb("mixT", [128, 8, T], BF16)
            o32 = yg
            p = lambda s: pfx + s

            for t in range(NT):
                t0 = t * T
                for b in range(2):
                    slot = (2 * t + b) % 4
                    xtok = p(f"xs{slot}")
                    self.dma("sync", xs[slot][:], xin[t0 + b * 128: t0 + (b + 1) * 128, :], xtok, [f"dram_{xin.tensor.name}"], [xtok])
                    self.norm_block(sk, pfx, xs[slot][:], xtok, gmix, p("gmix"),
                                    hT[:, :, b * 128:(b + 1) * 128], p("hT"), [PS[0], PS[1]], ["ps0", "ps1"], scr)
                def proj(n0, ncols, pout, ptok):
                    for k in range(8):
                        self.mm(pout, Win[:, k, n0:n0 + ncols], hT[:, k, :], k == 0, k == 7,
                                [p("hT")] + WIN, [ptok], sig=(k == 7))
                for j in range(12 if self.cut >= 4 else (int(round((self.cut - 3) * 100)) if self.cut > 3 else 0)):
                    n0 = [0, 128, 256, 384, 512, 640, 768, 896, 1536, 1664, 1792, 1920][j]
                    pout = PS[2 + j % 2][:, 0:256]
                    ptok = f"ps{2 + j % 2}"
                    proj(n0, 128, pout, ptok)
                    import os
                    dbgv = os.environ.get("DBGV", "")
                    if j < 4:
                        if dbgv != "A":
                            self.act(uT[:, j, :], pout, AF.Copy, [ptok], [p("uT")])
                        if dbgv not in ("A", "B"):
                            self.cp(uTb[:, j, :], uT[:, j, :], [p("uT")], [p("uTb")], eng="pool")
                    elif j < 8:
                        self.act(qkT[:, j - 4, :], pout, AF.Copy, [ptok], [p("qkT")])
                    else:
                        self.act(srT[:, j - 8, :], pout, AF.Silu, [ptok], [p("srT")])
                if self.cut >= 4.2:
                    proj(2048, 16, PS[6][0:16, 0:256], "ps6")
                    self.act(glT[:], PS[6][0:16, 0:256], AF.Copy, ["ps6"], [p("glT")])
                for b in range(2 if self.cut >= 4.3 else 0):
                    for k in range(8):
                        self.mm(PS[4][:, :], hT[:, k, b * 128:(b + 1) * 128], Win[:, k, 1024:1536], k == 0, k == 7,
                                [p("hT")] + WIN, ["ps4"], sig=(k == 7))
                    self.act(vtm[:, b, :], PS[4][:, :], AF.Copy, ["ps4"], [p("vtm")])
                    for k in range(8):
                        self.mm(PS[5][:, 0:256], hT[:, k, b * 128:(b + 1) * 128], Win[:, k, 768:1024], k == 0, k == 7,
                                [p("hT")] + WIN, ["ps5"], sig=(k == 7))
                    self.cp(ktm[:, b, :], PS[5][:, 0:256], ["ps5"], [p("ktm")])
                S5 = p("s5st")

                def st1(k):
                    j = k // 4
                    pb = PS[2 + (k % 2)]; ptk = f"ps{2 + (k % 2)}"
                    self.mm(pb[:, 0:T], BT[0][:, k, :], uTb[:, j, :], True, True, [p("BT"), p("uTb")], [ptk], sig=False)
                    self.mm(pb[:, T:2 * T], BT[1][:, k, :], uTb[:, j, :], True, True, [p("BT"), p("uTb")], [ptk])

                def st2(k):
                    q = k % 2
                    pb = PS[2 + q]; ptk = f"ps{2 + q}"
                    bre = pb[:, 0:T]; bim = pb[:, T:2 * T]
                    cr = cosT[:, k, :]; sr_ = sinT[:, k, :]
                    self.tt(A[q][0][:], bre, cr, ALU.mult, [ptk, TB], [p(f"A{q}0")])
                    self.tt(A[q][1][:], bim, sr_, ALU.mult, [ptk, TB], [p(f"A{q}1")])
                    self.tt(A[q][2][:], bim, cr, ALU.mult, [ptk, TB], [p(f"A{q}2")])
                    self.tt(A[q][3][:], bre, sr_, ALU.mult, [ptk, TB], [p(f"A{q}3")])
                    self.tt(vv[q][0][:], A[q][0][:], A[q][1][:], ALU.add, [p(f"A{q}0"), p(f"A{q}1")], [p(f"vv{q}0")], eng="pool")
                    self.tt(vv[q][1][:], A[q][2][:], A[q][3][:], ALU.subtract, [p(f"A{q}2"), p(f"A{q}3")], [p(f"vv{q}1")], eng="pool")

                def st3a(k):
                    q = k % 2
                    magb = prm[:, MAG, k:k + 1].to_broadcast([128, T])
                    self.P.op("dve", lambda e, o=ss[q][0][:], d1=vv[q][0][:], ini=sre[:, k:k + 1], mg=magb:
                              e.tensor_tensor_scan(out=o, data0=mg, data1=d1, initial=ini, op0=ALU.mult, op1=ALU.add),
                              reads=[self.tok(p(f"vv{q}0")), self.tok(S5), self.tok(PT)], writes=[self.tok(p(f"ss{q}0"))])
                    self.P.op("dve", lambda e, o=ss[q][1][:], d1=vv[q][1][:], ini=sim[:, k:k + 1], mg=magb:
                              e.tensor_tensor_scan(out=o, data0=mg, data1=d1, initial=ini, op0=ALU.mult, op1=ALU.add),
                              reads=[self.tok(p(f"vv{q}1")), self.tok(S5), self.tok(PT)], writes=[self.tok(p(f"ss{q}1"))])
                    self.act(lre[:, k:k + 1], ss[q][0][:, T - 1:T], AF.Copy, [p(f"ss{q}0")], [p("lst")])
                    self.act(lim[:, k:k + 1], ss[q][1][:, T - 1:T], AF.Copy, [p(f"ss{q}1")], [p("lst")])

                def st3b(k):
                    q = k % 2
                    j, m = k // 4, k % 4
                    cr = cosT[:, k, :]; sr_ = sinT[:, k, :]
                    self.tt(pp[q][0][:], ss[q][0][:], cr, ALU.mult, [p(f"ss{q}0"), TB], [p(f"pp{q}0")])
                    self.tt(pp[q][1][:], ss[q][1][:], sr_, ALU.mult, [p(f"ss{q}1"), TB], [p(f"pp{q}1")], eng="pool")
                    self.tt(pp[q][2][:], ss[q][0][:], sr_, ALU.mult, [p(f"ss{q}0"), TB], [p(f"pp{q}2")], eng="pool")
                    self.tt(pp[q][3][:], ss[q][1][:], cr, ALU.mult, [p(f"ss{q}1"), TB], [p(f"pp{q}3")])
                    ytk = f"ps{6 + j % 2}"
                    yps = PS[6 + j % 2][:, 0:T]
                    self.mm(yps, CT[0][:, k, :], pp[q][0][:], m == 0, False, [p("CT"), p(f"pp{q}0")], [ytk], sig=False)
                    self.mm(yps, CT[1][:, k, :], pp[q][1][:], False, False, [p("CT"), p(f"pp{q}1")], [ytk], sig=False)
                    self.mm(yps, CT[2][:, k, :], pp[q][2][:], False, False, [p("CT"), p(f"pp{q}2")], [ytk], sig=False)
                    self.mm(yps, CT[2][:, k, :], pp[q][3][:], False, m == 3, [p("CT"), p(f"pp{q}3")], [ytk])
                    if m == 3:
                        self.stt(yj[:], uT[:, j, :], dsk[:, j:j + 1], yps, ALU.mult, ALU.add, [p("uT"), p("dsk"), ytk], [p("yj")])
                        self.act(g1[:], yj[:], AF.Square, [p("yj")], [p("g1")])
                        self.ts(g1[:], g1[:], 0.044715, 1.0, ALU.mult, ALU.add, [p("g1")], [p("g1")])
                        self.tt(g1[:], g1[:], yj[:], ALU.mult, [p("g1"), p("yj")], [p("g1")])
                        self.act(g2[:], g1[:], AF.Sigmoid, [p("g1")], [p("g2")], scale=1.5957691216057308)
                        self.tt(yg[:, j, :], yj[:], g2[:], ALU.mult, [p("yj"), p("g2")], [p("yg")])
                        self.cp(ygb[:, j, :], yg[:, j, :], [p("yg")], [p("ygb")], eng="pool")

                if self.cut >= 5:
                    st1(0)
                    for k in range(16):
                        if k + 1 < 16:
                            st1(k + 1)
                        if k >= 1:
                            st3a(k - 1)
                        st2(k)
                        if k >= 1:
                            st3b(k - 1)
                    st3a(15)
                    st3b(15)
                if self.cut < 5:
                    self.emit_E(t, t0, xs, mixT, Wout, xout, p)
                    continue
                cT_ = cosT[:, :, T - 1]; sT_ = sinT[:, :, T - 1]
                self.tt(ctmp[:, 0, :], lre[:], cT_, ALU.mult, [p("lst"), TB], [p("ctmp")])
                self.tt(ctmp[:, 1, :], lim[:], sT_, ALU.mult, [p("lst"), TB], [p("ctmp")])
                self.tt(ctmp[:, 2, :], lre[:], sT_, ALU.mult, [p("lst"), TB], [p("ctmp")])
                self.tt(ctmp[:, 3, :], lim[:], cT_, ALU.mult, [p("lst"), TB], [p("ctmp")])
                self.tt(sre[:], ctmp[:, 0, :], ctmp[:, 1, :], ALU.subtract, [p("ctmp")], [S5])
                self.tt(sim[:], ctmp[:, 2, :], ctmp[:, 3, :], ALU.add, [p("ctmp")], [S5])
                Y2 = [p(f"y2{n}") for n in range(4)]
                SQ = [p(f"sq{n}") for n in range(4)]
                for n in range(4):
                    zp = PS[2 + n][:, 0:T]
                    for j in range(4):
                        self.mm(zp, Wglu[:, j, n * 128:(n + 1) * 128], ygb[:, j, :], j == 0, j == 3,
                                [p("Wglu"), p("ygb")], [f"ps{2 + n}"], sig=(j == 3))
                for n in range(4):
                    self.act(y2[:, n, :], PS[2 + n][:, 0:T], AF.Sigmoid, [f"ps{2 + n}", p("bglu")], [Y2[n]],
                             bias=bglu[:, n:n + 1])
                for n in range(4):
                    self.tt(y2[:, n, :], yg[:, n, :], y2[:, n, :], ALU.mult, [p("yg"), Y2[n]], [Y2[n]])
                for n in range(4):
                    self.act(sq[:, n, :], y2[:, n, :], AF.Square, [Y2[n]], [SQ[n]])
                msp = PS[6][:, 0:T]
                for n in range(4):
                    self.mm(msp, self.avg512[:], sq[:, n, :], n == 0, n == 3, ["avg512", SQ[n]], ["ps6"], sig=(n == 3))
                self.ts(msp, msp, 1.0, EPS, ALU.mult, ALU.add, ["ps6"], ["ps6"])
                self.act(msp, msp, AF.Sqrt, ["ps6"], ["ps6"])
                self.P.op("dve", lambda e, o=msp: e.reciprocal(out=o, in_=o), reads=[self.tok("ps6")], writes=[self.tok("ps6")])
                for n in range(4):
                    self.stt(mixT[:, n, :], y2[:, n, :], gs5[:, n:n + 1], msp, ALU.mult, ALU.mult,
                             [Y2[n], p("gs5"), "ps6"], [p(f"mixT{n}")])
                if self.cut < 5.1:
                    self.emit_E(t, t0, xs, mixT, Wout, xout, p)
                    continue
                LT = [Y2[0], Y2[1]]
                for b in range(2):
                    bc = slice(b * 128, (b + 1) * 128)
                    zb = PS[7 - b]; ztk = f"ps{7 - b}"
                    zp = zb[:, 0:256]
                    self.mm(zp, glT[:, bc], Wa2[:], True, False, [p("glT"), p("Wa2")], [ztk], sig=False)
                    self.mm(zp, self.ones1[:], ba2[:], False, True, ["ones1", p("ba2")], [ztk])
                    self.act(ltm[:, b, :], zp, AF.Exp, [ztk], [LT[b]], scale=-1.0)
                    self.act(ltm[:, b, :], ltm[:, b, :], AF.Ln, [LT[b]], [LT[b]], bias=1.0)
                    for hp in range(2):
                        cb = PS[2 + 2 * b + hp]; ctk = f"ps{2 + 2 * b + hp}"
                        self.mm(cb[:, 0:128], ltm[:, b, hp * 128:(hp + 1) * 128], self.triM[:], True, True,
                                [LT[b], "triM"], [ctk])
                    rp = zb[:, 256:512]
                    self.mm(rp, self.triU[:], ltm[:, b, :], True, True, ["triU", LT[b]], [ztk])
                    for hp in range(2):
                        cb = PS[2 + 2 * b + hp]; ctk = f"ps{2 + 2 * b + hp}"
                        self.act(cb[:, 128:256], cb[:, 0:128], AF.Exp, [ctk], [ctk])
                        self.act(cb[:, 256:384], cb[:, 0:128], AF.Exp, [ctk], [ctk], scale=-1.0)
                    self.act(rp, rp, AF.Exp, [ztk], [ztk])
                    for hp in range(2):
                        cb = PS[2 + 2 * b + hp]; ctk = f"ps{2 + 2 * b + hp}"
                        Eq = cb[:, 128:256]; Ek = cb[:, 256:384]
                        self.ts(dec[:, hp, 2 * b:2 * b + 2], Eq.rearrange("p (c i) -> p c i", c=2)[:, :, 63], 1.0, None,
                                ALU.mult, None, [ctk], [p("dec")])
                        self.stt(qtT[:, hp, bc], qkT[:, hp, bc], 0.125, Eq, ALU.mult, ALU.mult,
                                 [p("qkT"), ctk], [p("qtT")])
                        for i in range(2):
                            hs = slice(i * 64, (i + 1) * 64)
                            self.tt(ktTz[i][hs, hp, bc], qkT[hs, 2 + hp, bc], Ek[hs, :], ALU.mult,
                                    [p("qkT"), ctk], [p("ktT")])
                    for i in range(2):
                        hs = slice(i * 64, (i + 1) * 64)
                        self.tt(kendz[i][hs, b, :], ktm[hs, b, :], rp[hs, :], ALU.mult,
                                [p("ktm"), ztk], [p("kend")])
                oP = [PS[6], PS[7]]
                if self.cut < 5.2:
                    self.emit_E(t, t0, xs, mixT, Wout, xout, p)
                    continue
                for b in range(2):
                    bc0 = b * 128
                    sbk = 2 + 2 * b; stk = f"ps{sbk}"
                    ubk = [3 + 2 * b, 2 + 2 * b]
                    for h in range(4):
                        hp = h // 2
                        self.mm(PS[sbk][:, h * 128:(h + 1) * 128], ktTz[h % 2][:, hp, bc0:bc0 + 128],
                                qtT[:, hp, bc0:bc0 + 128], True, True, [p("ktT"), p("qtT")], [stk], sig=(h == 3))
                    self.tt(scTh[:, :, :], PS[sbk][:, :].rearrange("p (h i) -> p h i", h=4),
                            self.maskC[:].unsqueeze(1).to_broadcast([128, 4, 128]), ALU.mult, [stk, "maskC"], [p("scTh")])
                    for c in range(2):
                        g = 2 * b + c
                        c0 = bc0 + c * 64
                        utk = f"ps{ubk[c]}"
                        for h in range(4):
                            hp = h // 2
                            self.mm(PS[ubk[c]][:, h * 128:(h + 1) * 128],
                                    kendz[c][:, b, hp * 128:(hp + 1) * 128],
                                    vtm[:, b, h * 128:(h + 1) * 128], True, True,
                                    [p("kend"), p("vtm")], [utk], sig=(h == 3))
                        rin = (g + 1) % 2
                        for h in range(4):
                            hp = h // 2
                            ob = oP[hp][:, (h % 2) * 256 + c0:(h % 2) * 256 + c0 + 64]
                            otk = f"ps{6 + hp}"
                            self.mm(ob, Sgbz[rin][h % 2][:, hp, :], qtT[:, hp, c0:c0 + 64], True, False,
                                    [p(f"Sgb{rin}"), p("qtT")], [otk], sig=False)
                            self.mm(ob, vtm[:, b, h * 128:(h + 1) * 128],
                                    scTh[:, h, c * 64:(c + 1) * 64], False, True,
                                    [p("vtm"), p("scTh")], [otk])
                        for h in range(4):
                            hp, po = h // 2, (h % 2) * 64
                            hs = slice(po, po + 64)
                            self.stt(Sg[hs, hp, :], Sg[hs, hp, :], dec[hs, hp, g:g + 1],
                                     PS[ubk[c]][hs, h * 128:(h + 1) * 128], ALU.mult, ALU.add,
                                     [p("Sg"), p("dec"), utk], [p("Sg")])
                        for i in range(2):
                            hs = slice(i * 64, (i + 1) * 64)
                            self.cp(Sgbz[g % 2][i][hs, :, :], Sg[hs, :, :], [p("Sg")], [p(f"Sgb{g % 2}")], eng="act")
                if self.cut < 5.5:
                    self.emit_E(t, t0, xs, mixT, Wout, xout, p)
                    continue
                for hp in range(2):
                    self.act(o32[:, 2 * hp:2 * hp + 2, :], oP[hp][:, :].rearrange("p (h t) -> p h t", h=2), AF.Copy,
                             [f"ps{6 + hp}"], [p("yg")])
                self.act(sq[:], o32[:], AF.Square, [p("yg")], SQ)
                for h in range(4):
                    self.mm(PS[2 + h][:, 0:256], self.avg128[:], sq[:, h, :], True, True, ["avg128", SQ[h]], [f"ps{2 + h}"])
                for h in range(4):
                    pb = PS[2 + h][:, 0:256]
                    self.ts(pb, pb, 1.0, EPS, ALU.mult, ALU.add, [f"ps{2 + h}"], [f"ps{2 + h}"])
                for h in range(4):
                    pb = PS[2 + h][:, 0:256]
                    self.act(pb, pb, AF.Sqrt, [f"ps{2 + h}"], [f"ps{2 + h}"])
                for h in range(4):
                    pb = PS[2 + h][:, 0:256]
                    self.P.op("dve", lambda e, o=pb: e.reciprocal(out=o, in_=o), reads=[self.tok(f"ps{2 + h}")],
                              writes=[self.tok(f"ps{2 + h}")])
                for h in range(4):
                    pb = PS[2 + h][:, 0:256]
                    self.stt(pb, o32[:, h, :], ggla[:, h:h + 1], pb, ALU.mult, ALU.mult,
                             [p("yg"), p("ggla"), f"ps{2 + h}"], [f"ps{2 + h}"])
                for h in range(4):
                    pb = PS[2 + h][:, 0:256]
                    self.tt(mixT[:, 4 + h, :], pb, srT[:, h, :], ALU.mult, [f"ps{2 + h}", p("srT")], [p(f"mixT{4 + h}")])
                self.emit_E(t, t0, xs, mixT, Wout, xout, p)

    def emit_E(self, t, t0, xs, mixT, Wout, xout, p):
        PS = self.psum
        for b in range(2):
            slot = (2 * t + b) % 4
            xtok = p(f"xs{slot}")
            for half in range(2):
                pb = PS[half]
                for k in range(8):
                    self.mm(pb[:, :], mixT[:, k, b * 128:(b + 1) * 128], Wout[:, k, half * 512:(half + 1) * 512],
                            k == 0, k == 7, [p(f"mixT{k}"), p("Wout")], [f"ps{half}"], sig=(k == 7))
                self.tt(xs[slot][:, half * 512:(half + 1) * 512], pb[:, :], xs[slot][:, half * 512:(half + 1) * 512],
                        ALU.add, [f"ps{half}", xtok], [xtok])
            key = ("yout" if xout.tensor.name == "y" else p("st")) + str(b)
            self.dma("pool", xout[t0 + b * 128:t0 + (b + 1) * 128, :], xs[slot][:], key, [xtok],
                     [f"dram_{xout.tensor.name}"])

    def ffn(self, l, xin, xout, final):
        nc, dr, L = self.nc, self.dr, self.L
        pfx = f"f{l}_"
        p = lambda s: pfx + s
        PS = self.psum
        moe = (l == 1)
        ST = min(L, 2048)
        NST = L // ST
        NB = ST // 128
        NTL = ST // 256
        if moe:
            experts = list(range(NE)); FC = D_FFE // 128
        else:
            experts = [0]; FC = D_FF // 128
        slices = []
        for e in experts:
            c = 0
            while c < FC:
                n = min(4, FC - c)
                slices.append((e, c, n))
                c += n
        self.fence()
        with contextlib.ExitStack() as sk:
            sb = lambda name, shape, dt=F32: self.sb(sk, pfx + name, shape, dt)
            xacc = sb("xacc", [128, NB, D])
            hT = sb("hT", [128, 8, ST], BF16)
            gffn = sb("gffn", [128, 8]); self.load_colvec(gffn[:], dr["norm_ffn"][l], p("gffn"), p("gffn"))
            scr = {"stat": sb("stat", [128, 4]), "junk": sb("junk", [128, D], BF16), "xn": sb("xn", [128, D])}
            Wg = [sb(f"Wg{i}", [128, 8, 512], BF16) for i in range(2)]
            Wu = [sb(f"Wu{i}", [128, 8, 512], BF16) for i in range(2)]
            Wd = [sb(f"Wd{i}", [128, 4, D], BF16) for i in range(2)]
            sg = [sb(f"sg{i}", [128, 256]) for i in range(3)]
            aT = [sb(f"aT{i}", [128, 256], BF16) for i in range(6)]
            gates = sb("gates", [128, NB, NE])
            if moe:
                h32 = sb("h32", [128, 8, 128])
                Wr = sb("Wr", [128, 8, NE])
                self.dma("sync", Wr[:], dr["moe_w_router"][0].rearrange("(k q) e -> q k e", q=128), p("Wr"), [], [p("Wr")])
                lg = sb("lg", [128, 8]); mx = sb("mx", [128, 8]); rt = sb("rt", [128, 8]); gt = sb("gt", [128, 8])
            if final:
                gfin = sb("gfin", [128, D])
                self.dma("sync", gfin[:], dr["norm_final"].partition_broadcast(128), p("gfin"), [], [p("gfin")])
                yo = [sb(f"yo{i}", [128, D]) for i in range(2)]

            def wsrc(kind, e):
                if moe:
                    nm = {"g": "moe_w_gate", "u": "moe_w_up", "d": "moe_w_down"}[kind]
                    return dr[nm][0, e]
                nm = {"g": "ffn_w_gate", "u": "ffn_w_up", "d": "ffn_w_down"}[kind]
                return dr[nm][0]

            def load_slice(si, slot):
                e, c, n = slices[si]
                f0 = c * 128
                self.dma("pool", Wg[slot][:, :, 0:n * 128],
                         wsrc("g", e).rearrange("(k q) f -> q k f", q=128)[:, :, f0:f0 + n * 128],
                         p(f"Wg{slot}"), [], [p(f"Wg{slot}")])
                self.dma("pool", Wu[slot][:, :, 0:n * 128],
                         wsrc("u", e).rearrange("(k q) f -> q k f", q=128)[:, :, f0:f0 + n * 128],
                         p(f"Wu{slot}"), [], [p(f"Wu{slot}")])
                self.dma("pool", Wd[slot][:, 0:n, :],
                         wsrc("d", e)[f0:f0 + n * 128, :].rearrange("(c q) d -> q c d", q=128),
                         p(f"Wd{slot}"), [], [p(f"Wd{slot}")])

            for s in range(NST):
                r0 = s * ST
                load_slice(0, 0)
                for b in range(NB):
                    xtok = p(f"xacc{b}")
                    self.dma("sync", xacc[:, b, :], xin[r0 + b * 128:r0 + (b + 1) * 128, :], p(f"xl{b % 4}"),
                             [f"dram_{xin.tensor.name}"], [xtok])
                    self.norm_block(sk, pfx, xacc[:, b, :], xtok, gffn, p("gffn"),
                                    hT[:, :, b * 128:(b + 1) * 128], p("hT"), [PS[6], PS[7]], ["ps6", "ps7"], scr,
                                    h32_dst=(h32 if moe else None))
                    if moe:
                        lp = PS[5][:, 0:NE]
                        for k in range(8):
                            self.mm(lp, h32[:, k, :], Wr[:, k, :], k == 0, k == 7, [p("hT32"), p("Wr")], ["ps5"], sig=(k == 7))
                        G = p("gt")
                        self.cp(lg[:], lp, ["ps5"], [G])
                        self.P.op("dve", lambda e: e.max(out=mx[:], in_=lg[:]), reads=[self.tok(G)], writes=[self.tok(G)])
                        self.tt(rt[:, 0:1], mx[:, 1:2], mx[:, 0:1], ALU.subtract, [G], [G])
                        self.act(rt[:, 1:2], rt[:, 0:1], AF.Exp, [G], [G])
                        self.ts(rt[:, 2:3], rt[:, 1:2], 1.0, None, ALU.add, None, [G], [G])
                        self.P.op("dve", lambda e: e.reciprocal(out=rt[:, 3:4], in_=rt[:, 2:3]), reads=[self.tok(G)], writes=[self.tok(G)])
                        self.tt(rt[:, 4:5], rt[:, 1:2], rt[:, 3:4], ALU.mult, [G], [G])
                        self.ts(gt[:], lg[:], mx[:, 0:1], rt[:, 3:4], ALU.is_equal, ALU.mult, [G], [G])
                        self.ts(lg[:], lg[:], mx[:, 1:2], rt[:, 4:5], ALU.is_equal, ALU.mult, [G], [G])
                        self.tt(gates[:, b, :], gt[:], lg[:], ALU.add, [G], [p("gates")])
                units = [(si, tl, cc) for si, (e_, c_, n_) in enumerate(slices) for tl in range(NTL) for cc in range(n_)]
                NU = len(units)
                loaded = {0}

                def emit_gu(u):
                    si, tl, cc = units[u]
                    e, c, n = slices[si]
                    slot = si % 2
                    WT = [p(f"Wg{slot}"), p(f"Wu{slot}"), p(f"Wd{slot}")]
                    tc = slice(tl * 256, (tl + 1) * 256)
                    gb = PS[4 + u % 4]
                    gtk = f"ps{4 + u % 4}"
                    gp = gb[:, 0:256]; up = gb[:, 256:512]
                    for k in range(8):
                        self.mm(gp, Wg[slot][:, k, cc * 128:(cc + 1) * 128], hT[:, k, tc], k == 0, k == 7,
                                [p("hT"), WT[0]], [gtk], sig=False)
                    for k in range(8):
                        self.mm(up, Wu[slot][:, k, cc * 128:(cc + 1) * 128], hT[:, k, tc], k == 0, k == 7,
                                [p("hT"), WT[1]], [gtk], sig=(k == 7))
                    sgi = sg[u % 3]
                    self.act(sgi[:], gp, AF.Silu, [gtk], [p(f"sg{u % 3}")])
                    self.tt(aT[u % 6][:], sgi[:], up, ALU.mult, [p(f"sg{u % 3}"), gtk], [p(f"aT{u % 6}")])

                def emit_down(u):
                    si, tl, cc = units[u]
                    e, c, n = slices[si]
                    slot = si % 2
                    if si + 1 < len(slices) and (si + 1) not in loaded:
                        loaded.add(si + 1)
                        load_slice(si + 1, 1 - slot)
                    for b2 in range(2):
                        for half in range(2):
                            ai = b2 * 2 + half
                            self.mm(PS[ai][:, :], aT[u % 6][:, b2 * 128:(b2 + 1) * 128],
                                    Wd[slot][:, cc, half * 512:(half + 1) * 512], cc == 0, cc == n - 1,
                                    [p(f"aT{u % 6}"), p(f"Wd{slot}")], self.pst(ai), sig=(cc == n - 1))
                    if cc == n - 1:
                        for b2 in range(2):
                            b = tl * 2 + b2
                            xtok = p(f"xacc{b}")
                            for half in range(2):
                                ai = b2 * 2 + half
                                dst = xacc[:, b, half * 512:(half + 1) * 512]
                                if moe:
                                    self.stt(dst, PS[ai][:, :], gates[:, b, e:e + 1], dst, ALU.mult, ALU.add,
                                             self.pst(ai) + [p("gates"), xtok], [xtok])
                                else:
                                    self.tt(dst, PS[ai][:, :], dst, ALU.add, self.pst(ai) + [xtok], [xtok])

                for u in range(min(3, NU)):
                    emit_gu(u)
                for u in range(NU):
                    if u + 3 < NU:
                        emit_gu(u + 3)
                    emit_down(u)
                for b in range(NB):
                    xtok = p(f"xacc{b}")
                    rows = slice(r0 + b * 128, r0 + (b + 1) * 128)
                    if final:
                        o = b % 2
                        stt_ = scr["stat"]
                        self.act(scr["junk"][:], xacc[:, b, :], AF.Square, [xtok], [p("junk"), p("stat")], accum=stt_[:, 0:1])
                        self.rsqrt_(stt_[:, 1:2], stt_[:, 0:1], stt_[:, 2:3], [p("stat")], [p("stat")], mul=1.0 / D)
                        self.stt(yo[o][:], xacc[:, b, :], stt_[:, 1:2], gfin[:], ALU.mult, ALU.mult,
                                 [xtok, p("stat"), p("gfin")], [p(f"yo{o}")])
                        self.dma("sync", xout[rows, :], yo[o][:], f"yout{o}", [p(f"yo{o}")], [f"dram_{xout.tensor.name}"])
                    else:
                        key = ("yout" if xout.tensor.name == "y" else p("st")) + str(b % 2)
                        self.dma("sync", xout[rows, :], xacc[:, b, :], key, [xtok], [f"dram_{xout.tensor.name}"])


_CACHE = {}


def get_nc(L, stop_after=None):
    key = (L, stop_after)
    if key not in _CACHE:
        _CACHE[key] = K(L, stop_after).build()
    return _CACHE[key]


NAMES = ["norm_mix", "w_in", "s5_lambda_re", "s5_lambda_im", "s5_log_dt", "s5_b_re", "s5_b_im", "s5_c_re", "s5_c_im",
         "s5_d", "s5_w_glu", "s5_b_glu", "s5_out_norm", "gla_w_a2", "gla_b_a2", "gla_out_norm", "w_out", "norm_ffn",
         "ffn_w_gate", "ffn_w_up", "ffn_w_down", "moe_w_router", "moe_w_gate", "moe_w_up", "moe_w_down", "norm_final"]


def kernel(**inputs):
    x = np.ascontiguousarray(np.asarray(inputs["x"], dtype=np.float32))
    B, L, _ = x.shape
    nc = get_nc(L)
    shared = {n: np.ascontiguousarray(np.asarray(inputs[n], dtype=np.float32)) for n in NAMES}
    in_maps = []
    for b in range(B):
        m = dict(shared)
        m["x"] = x[b]
        in_maps.append(m)
    res = run_bass_kernel_spmd(nc, in_maps, core_ids=list(range(B)))
    return np.stack([np.asarray(r["y"], dtype=np.float32) for r in res.results], axis=0)
```

```python
import contextlib
import math
import numpy as np
import concourse.bass as bass
import concourse.mybir as mybir
from concourse.bass_utils import run_bass_kernel_spmd

F32 = mybir.dt.float32
BF16 = mybir.dt.bfloat16
I32 = mybir.dt.int32
AF = mybir.ActivationFunctionType
ALU = mybir.AluOpType

D = 1024
D_IN = 2064
D_FF = 2816
D_FFE = 3584
NE = 8
EPS = 1e-6
TWO_PI = 2.0 * math.pi

COMPUTE = ("pe", "act", "dve", "pool")
NO_SELF_WAIT = ()


class Tok:
    __slots__ = ("name", "w", "r")

    def __init__(self, name):
        self.name = name
        self.w = None
        self.r = {}


class Prog:
    def __init__(self, nc):
        self.nc = nc
        self.ops = {e: [] for e in ("pe", "act", "dve", "pool", "sync")}
        self.seq = {e: 0 for e in COMPUTE}
        self.waited = {e: {} for e in self.ops}
        self.dma_cnt = {}
        self.semkeys = []
        self.pending_sig = {e: False for e in COMPUTE}

    def _semkey(self, k):
        if k not in self.semkeys:
            self.semkeys.append(k)
        return k

    def _deps(self, reads, writes):
        deps = {}

        def add(k, v):
            if deps.get(k, 0) < v:
                deps[k] = v
        for t in reads:
            if t.w is not None:
                add(*t.w)
        for t in writes:
            if t.w is not None:
                add(*t.w)
            for k, v in t.r.items():
                add(k, v)
        return deps

    def _emit_waits(self, eng, deps):
        for k, v in deps.items():
            if k == eng and (eng == "pe" or eng in NO_SELF_WAIT):
                continue
            if self.waited[eng].get(k, 0) >= v:
                continue
            self.waited[eng][k] = v
            self.ops[eng].append(("wait", k, v))

    def op(self, eng, fn, reads=(), writes=(), sig=True):
        deps = self._deps(reads, writes)
        self._emit_waits(eng, deps)
        if sig:
            self.seq[eng] += 1
            comp = (eng, self.seq[eng])
            self.pending_sig[eng] = False
        else:
            comp = (eng, self.seq[eng] + 1)
            self.pending_sig[eng] = True
        self._semkey(eng)
        self.ops[eng].append(("op", fn, sig, eng))
        for t in writes:
            t.w = comp
            t.r = {}
        for t in reads:
            if t.r.get(comp[0], 0) < comp[1]:
                t.r[comp[0]] = comp[1]

    def frontier(self):
        f = {e: self.seq[e] + (1 if self.pending_sig[e] else 0) for e in COMPUTE if self.seq[e] or self.pending_sig[e]}
        f.update(self.dma_cnt)
        return f

    def dma(self, q, fn, key, reads=(), writes=()):
        deps = self._deps(reads, writes)
        k = self._semkey("dma_" + key)
        if self.dma_cnt.get(k, 0) > 0 and deps.get(k, 0) < self.dma_cnt[k]:
            deps[k] = self.dma_cnt[k]
        self._emit_waits(q, deps)
        self.dma_cnt[k] = self.dma_cnt.get(k, 0) + 16
        comp = (k, self.dma_cnt[k])
        self.ops[q].append(("dma", fn, k))
        for t in writes:
            t.w = comp
            t.r = {}
        for t in reads:
            if t.r.get(k, 0) < comp[1]:
                t.r[k] = comp[1]
        return comp

    def finish(self, final_waits):
        nc = self.nc
        for e in COMPUTE:
            assert not self.pending_sig[e], f"pending unsignalled op on {e}"
        with contextlib.ExitStack() as st:
            sems = {}
            for k in self.semkeys:
                sems[k] = st.enter_context(nc.semaphore(k))
            block = st.enter_context(nc.Block())

            def replay(ename, eng_obj, extra=()):
                for it in self.ops[ename]:
                    if it[0] == "wait":
                        eng_obj.wait_ge(sems[it[1]], it[2])
                    elif it[0] == "op":
                        ins = it[1](eng_obj)
                        if it[2]:
                            ins.then_inc(sems[it[3]], 1)
                    else:
                        ins = it[1](eng_obj)
                        ins.then_inc(sems[it[2]], 16)
                for k, v in extra:
                    eng_obj.wait_ge(sems[k], v)

            @block.tensor
            def _(e):
                replay("pe", e)

            @block.scalar
            def _(e):
                replay("act", e)

            @block.vector
            def _(e):
                replay("dve", e)

            @block.gpsimd
            def _(e):
                replay("pool", e, extra=final_waits)

            @block.sync
            def _(e):
                replay("sync", e)


class K:
    def __init__(self, L, stop_after=None, cut=99):
        self.L = L
        self.cut = cut
        self.stop_after = stop_after
        self.nc = bass.Bass("TRN2", target_bir_lowering=False)
        self.P = Prog(self.nc)
        self.toks = {}
        self.base = {}

    def tok(self, name):
        if name not in self.toks:
            t = Tok(name)
            t.r = dict(self.base)
            self.toks[name] = t
        return self.toks[name]

    def fence(self):
        self.base = self.P.frontier()

    def pst(self, i):
        return [f"ps{i}"]

    def mm(self, out, lhsT, rhs, start, stop, r, w, sig=True):
        self.P.op("pe", lambda e: e.matmul(out, lhsT, rhs, start=start, stop=stop),
                  reads=[self.tok(x) for x in r], writes=[self.tok(x) for x in w], sig=sig)

    def tr(self, out, in_, ident, r, w, sig=True):
        self.P.op("pe", lambda e: e.transpose(out, in_, ident),
                  reads=[self.tok(x) for x in r], writes=[self.tok(x) for x in w], sig=sig)

    def act(self, out, in_, func, r, w, bias=None, scale=None, accum=None):
        kw = {}
        if bias is not None:
            kw["bias"] = bias
        if scale is not None:
            kw["scale"] = scale
        if accum is not None:
            kw["accum_out"] = accum
        self.P.op("act", lambda e: e.activation(out=out, in_=in_, func=func, **kw),
                  reads=[self.tok(x) for x in r], writes=[self.tok(x) for x in w])

    def ts(self, out, in0, s1, s2, op0, op1, r, w, eng="dve"):
        if op1 is None:
            f = lambda e: e.tensor_scalar(out=out, in0=in0, scalar1=s1, scalar2=None, op0=op0)
        else:
            f = lambda e: e.tensor_scalar(out=out, in0=in0, scalar1=s1, scalar2=s2, op0=op0, op1=op1)
        self.P.op(eng, f, reads=[self.tok(x) for x in r], writes=[self.tok(x) for x in w])

    def stt(self, out, in0, scalar, in1, op0, op1, r, w):
        self.P.op("dve", lambda e: e.scalar_tensor_tensor(out=out, in0=in0, scalar=scalar, in1=in1,
                                                          op0=op0, op1=op1),
                  reads=[self.tok(x) for x in r], writes=[self.tok(x) for x in w])

    def tt(self, out, in0, in1, op, r, w, eng="dve"):
        self.P.op(eng, lambda e: e.tensor_tensor(out=out, in0=in0, in1=in1, op=op),
                  reads=[self.tok(x) for x in r], writes=[self.tok(x) for x in w])

    def cp(self, out, in_, r, w, eng="dve"):
        if eng == "act":
            return self.act(out, in_, AF.Copy, r, w)
        if eng == "dve" and any(x.startswith("ps") for x in r):
            return self.ts(out, in_, 1.0, None, ALU.mult, None, r, w)
        self.P.op(eng, lambda e: e.tensor_copy(out=out, in_=in_),
                  reads=[self.tok(x) for x in r], writes=[self.tok(x) for x in w])

    def memset(self, ap, val, w, eng="pool"):
        self.P.op(eng, lambda e: e.memset(ap, val), writes=[self.tok(x) for x in w])

    def dma(self, q, out, in_, key, r, w, **kw):
        return self.P.dma(q, lambda e: e.dma_start(out=out, in_=in_, **kw), key,
                          reads=[self.tok(x) for x in r], writes=[self.tok(x) for x in w])

    def rsqrt_(self, out, in_, tmp, r, w, eps=EPS, mul=1.0):
        self.ts(tmp, in_, mul, eps, ALU.mult, ALU.add, r, w)
        self.act(tmp, tmp, AF.Sqrt, w, w)
        self.P.op("dve", lambda e: e.reciprocal(out=out, in_=tmp),
                  reads=[self.tok(x) for x in w], writes=[self.tok(x) for x in w])

    def build(self):
        nc = self.nc
        L = self.L
        dr = {}

        def din(name, shape):
            dr[name] = nc.dram_tensor(name, list(shape), F32, kind="ExternalInput").ap()
        din("x", [L, D])
        din("norm_mix", [2, D]); din("w_in", [2, D, D_IN])
        din("s5_lambda_re", [2, 32, 64]); din("s5_lambda_im", [2, 32, 64]); din("s5_log_dt", [2, 32])
        din("s5_b_re", [2, 32, 64, 16]); din("s5_b_im", [2, 32, 64, 16])
        din("s5_c_re", [2, 32, 16, 64]); din("s5_c_im", [2, 32, 16, 64])
        din("s5_d", [2, 512]); din("s5_w_glu", [2, 512, 512]); din("s5_b_glu", [2, 512])
        din("s5_out_norm", [2, 512]); din("gla_w_a2", [2, 16, 256]); din("gla_b_a2", [2, 256])
        din("gla_out_norm", [2, 512]); din("w_out", [2, D, D]); din("norm_ffn", [2, D])
        din("ffn_w_gate", [1, D, D_FF]); din("ffn_w_up", [1, D, D_FF]); din("ffn_w_down", [1, D_FF, D])
        din("moe_w_router", [1, D, NE]); din("moe_w_gate", [1, NE, D, D_FFE])
        din("moe_w_up", [1, NE, D, D_FFE]); din("moe_w_down", [1, NE, D_FFE, D])
        din("norm_final", [D])
        self.dr = dr
        y = nc.dram_tensor("y", [L, D], F32, kind="ExternalOutput").ap()
        s1 = nc.dram_tensor("scr1", [L, D], F32, kind="Internal").ap()
        s2 = nc.dram_tensor("scr2", [L, D], F32, kind="Internal").ap()
        sa = self.stop_after
        with contextlib.ExitStack() as st:
            self.st = st
            self.psum = [st.enter_context(nc.psum_tensor(f"ps{i}", [128, 512], F32)) for i in range(8)]
            self.consts()
            if sa == "mix0":
                self.mixer(0, dr["x"], y)
            elif sa == "ffn0":
                self.mixer(0, dr["x"], s1)
                self.ffn(0, s1, y, final=False)
            elif sa == "mix1":
                self.mixer(0, dr["x"], s1)
                self.ffn(0, s1, s2, final=False)
                self.mixer(1, s2, y)
            elif sa == "ffnonly":
                self.ffn(0, dr["x"], y, final=False)
            elif sa == "moeonly":
                self.ffn(1, dr["x"], y, final=True)
            else:
                self.mixer(0, dr["x"], s1)
                self.ffn(0, s1, s2, final=False)
                self.mixer(1, s2, s1)
                self.ffn(1, s1, y, final=True)
            fw = [(k, v) for k, v in self.P.dma_cnt.items() if k.startswith("dma_yout")]
            self.P.finish(fw)
        return nc

    def sb(self, stack, name, shape, dt=F32):
        return stack.enter_context(self.nc.sbuf_tensor(name, list(shape), dt))

    def consts(self):
        st = self.st
        self.ident = self.sb(st, "ident", [128, 128])
        self.onesf = self.sb(st, "onesf", [128, 128])
        self.memset(self.onesf[:], 1.0, ["onesf"])
        self.P.op("pool", lambda e: e.affine_select(out=self.ident[:], in_=self.onesf[:], pattern=[[-1, 128]],
                                                    compare_op=ALU.is_equal, fill=0.0, base=0,
                                                    channel_multiplier=1),
                  reads=[self.tok("onesf")], writes=[self.tok("ident")])
        self.triM = self.sb(st, "triM", [128, 128])
        self.triU = self.sb(st, "triU", [128, 128])
        self.maskC = self.sb(st, "maskC", [128, 128])
        self.blk = self.sb(st, "blkm", [128, 128])
        self.memset(self.blk[:], 0.0, ["blkm"])
        self.memset(self.blk[0:64, 0:64], 1.0, ["blkm"])
        self.memset(self.blk[64:128, 64:128], 1.0, ["blkm"])
        self.P.op("pool", lambda e: e.affine_select(out=self.maskC[:], in_=self.blk[:], pattern=[[1, 128]],
                                                    compare_op=ALU.is_ge, fill=0.0, base=0,
                                                    channel_multiplier=-1),
                  reads=[self.tok("blkm")], writes=[self.tok("maskC")])
        self.ts(self.triM[:], self.maskC[:], -1.0 / 16.0, None, ALU.mult, None, ["maskC"], ["triM"], eng="pool")
        self.P.op("pool", lambda e: e.affine_select(out=self.triU[:], in_=self.blk[:], pattern=[[-1, 128]],
                                                    compare_op=ALU.is_gt, fill=0.0, base=0,
                                                    channel_multiplier=1),
                  reads=[self.tok("blkm")], writes=[self.tok("triU")])
        self.ts(self.triU[:], self.triU[:], -1.0 / 16.0, None, ALU.mult, None, ["triU"], ["triU"], eng="pool")
        self.avg512 = self.sb(st, "avg512", [128, 128], BF16)
        self.avg128 = self.sb(st, "avg128", [128, 128], BF16)
        self.memset(self.avg512[:], 1.0 / 512.0, ["avg512"])
        self.memset(self.avg128[:], 1.0 / 128.0, ["avg128"])
        self.ones1 = self.sb(st, "ones1", [1, 128])
        self.memset(self.ones1[:], 1.0, ["ones1"])

    def norm_block(self, stk, pfx, xs_ap, xtok, gT, gtok, hT_dst, hT_tok, ps2, pstoks, scr, h32_dst=None):
        stt_ = scr["stat"]
        self.act(scr["junk"][:], xs_ap, AF.Square, [xtok], [pfx + "junk", pfx + "stat"], accum=stt_[:, 0:1])
        self.rsqrt_(stt_[:, 1:2], stt_[:, 0:1], stt_[:, 2:3], [pfx + "stat"], [pfx + "stat"], mul=1.0 / D)
        self.ts(scr["xn"][:], xs_ap, stt_[:, 1:2], None, ALU.mult, None, [xtok, pfx + "stat"], [pfx + "xn"])
        for k in range(8):
            self.tr(ps2[k // 4][:, (k % 4) * 128:(k % 4 + 1) * 128], scr["xn"][:, k * 128:(k + 1) * 128],
                    self.ident[:], [pfx + "xn", "ident"], [pstoks[k // 4]], sig=(k % 4 == 3))
        for hb in range(2):
            self.tt(hT_dst[:, hb * 4:(hb + 1) * 4, :],
                    ps2[hb][:, :].rearrange("p (k t) -> p k t", k=4),
                    gT[:, hb * 4:(hb + 1) * 4].unsqueeze(2).to_broadcast([128, 4, 128]),
                    ALU.mult, [pstoks[hb], gtok], [hT_tok])
            if h32_dst is not None:
                self.tt(h32_dst[:, hb * 4:(hb + 1) * 4, :],
                        ps2[hb][:, :].rearrange("p (k t) -> p k t", k=4),
                        gT[:, hb * 4:(hb + 1) * 4].unsqueeze(2).to_broadcast([128, 4, 128]),
                        ALU.mult, [pstoks[hb], gtok], [hT_tok + "32"])

    def load_colvec(self, dst, src_flat, key, tokn):
        self.dma("sync", dst, src_flat.rearrange("(k q) -> q k", q=128), key, [], [tokn],
                 allow_slow_non_contiguous=True)

    def mixer(self, l, xin, xout):
        nc, dr, L = self.nc, self.dr, self.L
        T = 256
        NT = L // T
        pfx = f"m{l}_"
        PS = self.psum
        self.fence()
        with contextlib.ExitStack() as sk:
            sb = lambda name, shape, dt=F32: self.sb(sk, pfx + name, shape, dt)
            Win = sb("Win", [128, 8, D_IN], BF16)
            for c in range(3):
                self.dma("pool", Win[:, :, c * 688:(c + 1) * 688],
                         dr["w_in"][l].rearrange("(k q) n -> q k n", q=128)[:, :, c * 688:(c + 1) * 688],
                         pfx + f"Win{c}", [], [pfx + f"Win{c}"])
            WIN = [pfx + f"Win{c}" for c in range(3)]
            Wout = sb("Wout", [128, 8, D], BF16)
            self.dma("pool", Wout[:], dr["w_out"][l].rearrange("(k q) n -> q k n", q=128), pfx + "Wout", [], [pfx + "Wout"])
            Wglu = sb("Wglu", [128, 4, 512], BF16)
            self.dma("pool", Wglu[:], dr["s5_w_glu"][l].rearrange("(k q) n -> q k n", q=128), pfx + "Wglu", [], [pfx + "Wglu"])
            Wa2 = sb("Wa2", [16, 256])
            self.dma("sync", Wa2[:], dr["gla_w_a2"][l], pfx + "Wa2", [], [pfx + "Wa2"])
            ba2 = sb("ba2", [1, 256])
            self.dma("sync", ba2[:], dr["gla_b_a2"][l:l + 1, :], pfx + "ba2", [], [pfx + "ba2"])
            gmix = sb("gmix", [128, 8]); self.load_colvec(gmix[:], dr["norm_mix"][l], pfx + "gmix", pfx + "gmix")
            dsk = sb("dsk", [128, 4]); self.load_colvec(dsk[:], dr["s5_d"][l], pfx + "dsk", pfx + "dsk")
            bglu = sb("bglu", [128, 4]); self.load_colvec(bglu[:], dr["s5_b_glu"][l], pfx + "bglu", pfx + "bglu")
            gs5 = sb("gs5", [128, 4]); self.load_colvec(gs5[:], dr["s5_out_norm"][l], pfx + "gs5", pfx + "gs5")
            ggla = sb("ggla", [128, 4]); self.load_colvec(ggla[:], dr["gla_out_norm"][l], pfx + "ggla", pfx + "ggla")
            prm = sb("prm", [128, 24, 16])
            PT = pfx + "prm"

            def pr(i):
                return prm[:, i, :]
            LR, LI, DT, MAG, TH, N_, SN, CS, ABR, ABI, DEN, FRE, FIM, T1, T2, T3 = range(16)
            self.dma("sync", pr(LR), dr["s5_lambda_re"][l].rearrange("(k two) p -> (two p) k", two=2), pfx + "lr", [], [PT],
                     allow_slow_non_contiguous=True)
            self.dma("sync", pr(LI), dr["s5_lambda_im"][l].rearrange("(k two) p -> (two p) k", two=2), pfx + "li", [], [PT],
                     allow_slow_non_contiguous=True)
            for two in range(2):
                self.dma("sync", prm[two * 64:(two + 1) * 64, DT, :],
                         dr["s5_log_dt"][l].rearrange("(k two) -> two k", two=2)[two].partition_broadcast(64),
                         pfx + f"dt{two}", [], [PT], allow_slow_non_contiguous=True)
            self.ts(pr(LR), pr(LR), -1e-4, None, ALU.min, None, [PT], [PT])
            self.act(pr(DT), pr(DT), AF.Exp, [PT], [PT])
            self.tt(pr(T1), pr(LR), pr(DT), ALU.mult, [PT], [PT])
            self.act(pr(MAG), pr(T1), AF.Exp, [PT], [PT])
            self.tt(pr(TH), pr(LI), pr(DT), ALU.mult, [PT], [PT])
            ni = sb("ni", [128, 16], I32)

            def reduce_angle(ap, shape_ni, tmpf, toks):
                self.ts(tmpf, ap, 1.0 / TWO_PI, None, ALU.mult, None, toks, toks)
                self.cp(shape_ni, tmpf, toks, toks)
                self.cp(tmpf, shape_ni, toks, toks)
                self.stt(ap, tmpf, -TWO_PI, ap, ALU.mult, ALU.add, toks, toks)
                self.ts(tmpf, ap, math.pi, -TWO_PI, ALU.is_gt, ALU.mult, toks, toks)
                self.tt(ap, ap, tmpf, ALU.add, toks, toks)
                self.ts(tmpf, ap, -math.pi, TWO_PI, ALU.is_lt, ALU.mult, toks, toks)
                self.tt(ap, ap, tmpf, ALU.add, toks, toks)
            reduce_angle(pr(TH), ni[:], pr(T1), [PT])
            self.act(pr(SN), pr(TH), AF.Sin, [PT], [PT])
            self.ts(pr(T2), pr(TH), math.pi / 2, None, ALU.add, None, [PT], [PT])
            reduce_angle(pr(T2), ni[:], pr(T1), [PT])
            self.act(pr(CS), pr(T2), AF.Sin, [PT], [PT])
            self.tt(pr(ABR), pr(MAG), pr(CS), ALU.mult, [PT], [PT])
            self.tt(pr(ABI), pr(MAG), pr(SN), ALU.mult, [PT], [PT])
            self.ts(pr(ABR), pr(ABR), -1.0, None, ALU.add, None, [PT], [PT])
            self.tt(pr(DEN), pr(LR), pr(LR), ALU.mult, [PT], [PT])
            self.tt(pr(T1), pr(LI), pr(LI), ALU.mult, [PT], [PT])
            self.tt(pr(DEN), pr(DEN), pr(T1), ALU.add, [PT], [PT])
            self.P.op("dve", lambda e: e.reciprocal(out=pr(DEN), in_=pr(DEN)), reads=[self.tok(PT)], writes=[self.tok(PT)])
            self.tt(pr(T1), pr(ABR), pr(LR), ALU.mult, [PT], [PT])
            self.tt(pr(T2), pr(ABI), pr(LI), ALU.mult, [PT], [PT])
            self.tt(pr(T1), pr(T1), pr(T2), ALU.add, [PT], [PT])
            self.tt(pr(FRE), pr(T1), pr(DEN), ALU.mult, [PT], [PT])
            self.tt(pr(T1), pr(ABI), pr(LR), ALU.mult, [PT], [PT])
            self.tt(pr(T2), pr(ABR), pr(LI), ALU.mult, [PT], [PT])
            self.tt(pr(T1), pr(T1), pr(T2), ALU.subtract, [PT], [PT])
            self.tt(pr(FIM), pr(T1), pr(DEN), ALU.mult, [PT], [PT])
            cosT = sb("cosT", [128, 16, T])
            sinT = sb("sinT", [128, 16, T])
            TB = pfx + "tab"
            with contextlib.ExitStack() as s2:
              if self.cut >= 2:
                  iot = self.sb(s2, pfx + "iot", [128, T])
                  self.P.op("pool", lambda e: e.iota(iot[:], [[1, T]], base=1, channel_multiplier=0,
                                                     allow_small_or_imprecise_dtypes=True),
                            writes=[self.tok(pfx + "iot")])
                  tmpA = self.sb(s2, pfx + "tmpA", [128, 16, T])
                  tmpN = self.sb(s2, pfx + "tmpN", [128, 16, T], I32)
                  ang = self.sb(s2, pfx + "ang", [128, 16, T])
                  self.tt(ang[:], prm[:, TH, :].unsqueeze(2).to_broadcast([128, 16, T]),
                          iot[:].unsqueeze(1).to_broadcast([128, 16, T]), ALU.mult, [PT, pfx + "iot"], [TB])
                  fl = lambda a: a[:].rearrange("p k t -> p (k t)")
                  reduce_angle(fl(ang), fl(tmpN), fl(tmpA), [TB])
                  self.act(fl(sinT), fl(ang), AF.Sin, [TB], [TB])
                  self.ts(fl(ang), fl(ang), math.pi / 2, None, ALU.add, None, [TB], [TB])
                  reduce_angle(fl(ang), fl(tmpN), fl(tmpA), [TB])
                  self.act(fl(cosT), fl(ang), AF.Sin, [TB], [TB])
            self.fence()
            BT = [sb("BTre", [128, 16, 128], BF16), sb("BTim", [128, 16, 128], BF16)]
            CT = [sb("CTre", [128, 16, 128], BF16), sb("CTnre", [128, 16, 128], BF16), sb("CTnim", [128, 16, 128], BF16)]
            with contextlib.ExitStack() as s2:
                bp = [self.sb(s2, pfx + f"bp{i}", [128, 16, 128]) for i in range(2)]
                bb = [self.sb(s2, pfx + f"bb{i}", [128, 16, 128]) for i in range(2)]
                cpad = [self.sb(s2, pfx + f"cp{i}", [128, 16, 128]) for i in range(2)]
                tmpB = self.sb(s2, pfx + "tmpB", [128, 16, 128])
                for i, nm in enumerate(("s5_b_re", "s5_b_im")):
                    self.memset(bp[i][:], 0.0, [pfx + f"bp{i}"])
                    for two in range(2):
                        for m in range(4):
                            dst = bp[i][two * 64:(two + 1) * 64, :, :].rearrange("p (j m) c -> p j m c", m=4)[
                                :, :, m, m * 32 + two * 16: m * 32 + two * 16 + 16]
                            src = dr[nm][l].rearrange("(j m two) p c -> two m p j c", m=4, two=2)[two, m]
                            self.dma("sync", dst, src, pfx + f"bp{i}", [], [pfx + f"bp{i}"])
                for i, nm in enumerate(("s5_c_re", "s5_c_im")):
                    self.memset(cpad[i][:], 0.0, [pfx + f"cp{i}"])
                    for two in range(2):
                        for m in range(4):
                            r0 = m * 32 + two * 16
                            dst = cpad[i][r0:r0 + 16, :, :].rearrange("p (j m) c -> p j m c", m=4)[
                                :, :, m, two * 64:(two + 1) * 64]
                            src = dr[nm][l].rearrange("(j m two) c p -> two m c j p", m=4, two=2)[two, m]
                            self.dma("sync", dst, src, pfx + f"cp{i}", [], [pfx + f"cp{i}"])
                fre_b = prm[:, FRE, :].unsqueeze(2).to_broadcast([128, 16, 128])
                fim_b = prm[:, FIM, :].unsqueeze(2).to_broadcast([128, 16, 128])
                B0, B1 = pfx + "bp0", pfx + "bp1"
                self.tt(bb[0][:], bp[0][:], fre_b, ALU.mult, [B0, PT], [pfx + "bb0"])
                self.tt(tmpB[:], bp[1][:], fim_b, ALU.mult, [B1, PT], [pfx + "tmpB"])
                self.tt(bb[0][:], bb[0][:], tmpB[:], ALU.subtract, [pfx + "bb0", pfx + "tmpB"], [pfx + "bb0"])
                self.tt(bb[1][:], bp[1][:], fre_b, ALU.mult, [B1, PT], [pfx + "bb1"])
                self.tt(tmpB[:], bp[0][:], fim_b, ALU.mult, [B0, PT], [pfx + "tmpB"])
                self.tt(bb[1][:], bb[1][:], tmpB[:], ALU.add, [pfx + "bb1", pfx + "tmpB"], [pfx + "bb1"])
                cnt = 0
                for i in range(2 if self.cut >= 3 else 0):
                    for k in range(16):
                        pb = PS[cnt % 2]; ptk = f"ps{cnt % 2}"
                        cnt += 1
                        self.tr(pb[:, 0:128], bb[i][:, k, :], self.ident[:], [pfx + f"bb{i}", "ident"], [ptk])
                        self.cp(BT[i][:, k, :], pb[:, 0:128], [ptk], [pfx + "BT"], eng="act" if k % 2 else "dve")
                for i in range(2 if self.cut >= 3 else 0):
                    for k in range(16):
                        pb = PS[cnt % 2]; ptk = f"ps{cnt % 2}"
                        cnt += 1
                        self.tr(pb[:, 0:128], cpad[i][:, k, :], self.ident[:], [pfx + f"cp{i}", "ident"], [ptk])
                        if i == 0:
                            self.cp(CT[0][:, k, :], pb[:, 0:128], [ptk], [pfx + "CT"], eng="act")
                            self.ts(CT[1][:, k, :], pb[:, 0:128], -1.0, None, ALU.mult, None, [ptk], [pfx + "CT"])
                        else:
                            self.ts(CT[2][:, k, :], pb[:, 0:128], -1.0, None, ALU.mult, None, [ptk], [pfx + "CT"])
            self.fence()
            sre = sb("sre", [128, 16]); sim = sb("sim", [128, 16])
            lre = sb("lre", [128, 16]); lim = sb("lim", [128, 16])
            ctmp = sb("ctmp", [128, 4, 16])
            self.memset(sre[:], 0.0, [pfx + "s5st"]); self.memset(sim[:], 0.0, [pfx + "s5st"])
            Sg = sb("Sg", [128, 2, 128])
            Sgbz = [[sb(f"Sgbz{r}{i}", [128, 2, 128], BF16) for i in range(2)] for r in range(2)]
            self.memset(Sg[:], 0.0, [pfx + "Sg"])
            for r in range(2):
                for i in range(2):
                    self.memset(Sgbz[r][i][:], 0.0, [pfx + f"Sgb{r}"])
            xs = [sb(f"xs{i}", [128, D]) for i in range(4)]
            scr = {"stat": sb("stat", [128, 4]), "junk": sb("junk", [128, D], BF16), "xn": sb("xn", [128, D])}
            hT = sb("hT", [128, 8, T], BF16)
            uT = sb("uT", [128, 4, T]); uTb = sb("uTb", [128, 4, T], BF16)
            qkT = sb("qkT", [128, 4, T])
            srT = sb("srT", [128, 4, T])
            glT = sb("glT", [16, T])
            vtm = sb("vtm", [128, 2, 512], BF16)
            ktm = sb("ktm", [128, 2, 256])
            kendz = [sb(f"kendz{i}", [128, 2, 256], BF16) for i in range(2)]
            qtT = sb("qtT", [128, 2, T], BF16)
            ktTz = [sb(f"ktTz{i}", [128, 2, T], BF16) for i in range(2)]
            for i in range(2):
                self.memset(kendz[i][:], 0.0, [pfx + "kend"])
                self.memset(ktTz[i][:], 0.0, [pfx + "ktT"])
            dec = sb("dec", [128, 2, 4])
            A = [[sb(f"A{q}{i}", [128, T]) for i in range(4)] for q in range(2)]
            vv = [[sb(f"vv{q}{i}", [128, T]) for i in range(2)] for q in range(2)]
            ss = [[sb(f"ss{q}{i}", [128, T]) for i in range(2)] for q in range(2)]
            pp = [[sb(f"pp{q}{i}", [128, T], BF16) for i in range(4)] for q in range(2)]
            yj = sb("yj", [128, T]); g1 = sb("g1", [128, T]); g2 = sb("g2", [128, T])
            yg = sb("yg", [128, 4, T]); ygb = sb("ygb", [128, 4, T], BF16)
            y2 = sb("y2", [128, 4, T]); sq = sb("sq", [128, 4, T], BF16)
            scTh = sb("scTh", [128, 4, 128], BF16)
            ltm = y2[:, 0:2, :]
            mixT = sb("mixT", [128, 8, T], BF16)
            o32 = yg
            p = lambda s: pfx + s

            for t in range(NT):
                t0 = t * T
                for b in range(2):
                    slot = (2 * t + b) % 4
                    xtok = p(f"xs{slot}")
                    self.dma("sync", xs[slot][:], xin[t0 + b * 128: t0 + (b + 1) * 128, :], xtok, [f"dram_{xin.tensor.name}"], [xtok])
                    self.norm_block(sk, pfx, xs[slot][:], xtok, gmix, p("gmix"),
                                    hT[:, :, b * 128:(b + 1) * 128], p("hT"), [PS[0], PS[1]], ["ps0", "ps1"], scr)
                def proj(n0, ncols, pout, ptok):
                    for k in range(8):
                        self.mm(pout, Win[:, k, n0:n0 + ncols], hT[:, k, :], k == 0, k == 7,
                                [p("hT")] + WIN, [ptok], sig=(k == 7))
                for j in range(12 if self.cut >= 4 else (int(round((self.cut - 3) * 100)) if self.cut > 3 else 0)):
                    n0 = [0, 128, 256, 384, 512, 640, 768, 896, 1536, 1664, 1792, 1920][j]
                    pout = PS[2 + j % 2][:, 0:256]
                    ptok = f"ps{2 + j % 2}"
                    proj(n0, 128, pout, ptok)
                    import os
                    dbgv = os.environ.get("DBGV", "")
                    if j < 4:
                        if dbgv != "A":
                            self.act(uT[:, j, :], pout, AF.Copy, [ptok], [p("uT")])
                        if dbgv not in ("A", "B"):
                            self.cp(uTb[:, j, :], uT[:, j, :], [p("uT")], [p("uTb")], eng="pool")
                    elif j < 8:
                        self.act(qkT[:, j - 4, :], pout, AF.Copy, [ptok], [p("qkT")])
                    else:
                        self.act(srT[:, j - 8, :], pout, AF.Silu, [ptok], [p("srT")])
                if self.cut >= 4.2:
                    proj(2048, 16, PS[6][0:16, 0:256], "ps6")
                    self.act(glT[:], PS[6][0:16, 0:256], AF.Copy, ["ps6"], [p("glT")])
                for b in range(2 if self.cut >= 4.3 else 0):
                    for k in range(8):
                        self.mm(PS[4][:, :], hT[:, k, b * 128:(b + 1) * 128], Win[:, k, 1024:1536], k == 0, k == 7,
                                [p("hT")] + WIN, ["ps4"], sig=(k == 7))
                    self.act(vtm[:, b, :], PS[4][:, :], AF.Copy, ["ps4"], [p("vtm")])
                    for k in range(8):
                        self.mm(PS[5][:, 0:256], hT[:, k, b * 128:(b + 1) * 128], Win[:, k, 768:1024], k == 0, k == 7,
                                [p("hT")] + WIN, ["ps5"], sig=(k == 7))
                    self.cp(ktm[:, b, :], PS[5][:, 0:256], ["ps5"], [p("ktm")])
                S5 = p("s5st")

                def st1(k):
                    j = k // 4
                    pb = PS[2 + (k % 2)]; ptk = f"ps{2 + (k % 2)}"
                    self.mm(pb[:, 0:T], BT[0][:, k, :], uTb[:, j, :], True, True, [p("BT"), p("uTb")], [ptk], sig=False)
                    self.mm(pb[:, T:2 * T], BT[1][:, k, :], uTb[:, j, :], True, True, [p("BT"), p("uTb")], [ptk])

                def st2(k):
                    q = k % 2
                    pb = PS[2 + q]; ptk = f"ps{2 + q}"
                    bre = pb[:, 0:T]; bim = pb[:, T:2 * T]
                    cr = cosT[:, k, :]; sr_ = sinT[:, k, :]
                    self.tt(A[q][0][:], bre, cr, ALU.mult, [ptk, TB], [p(f"A{q}0")])
                    self.tt(A[q][1][:], bim, sr_, ALU.mult, [ptk, TB], [p(f"A{q}1")])
                    self.tt(A[q][2][:], bim, cr, ALU.mult, [ptk, TB], [p(f"A{q}2")])
                    self.tt(A[q][3][:], bre, sr_, ALU.mult, [ptk, TB], [p(f"A{q}3")])
                    self.tt(vv[q][0][:], A[q][0][:], A[q][1][:], ALU.add, [p(f"A{q}0"), p(f"A{q}1")], [p(f"vv{q}0")], eng="pool")
                    self.tt(vv[q][1][:], A[q][2][:], A[q][3][:], ALU.subtract, [p(f"A{q}2"), p(f"A{q}3")], [p(f"vv{q}1")], eng="pool")

                def st3a(k):
                    q = k % 2
                    magb = prm[:, MAG, k:k + 1].to_broadcast([128, T])
                    self.P.op("dve", lambda e, o=ss[q][0][:], d1=vv[q][0][:], ini=sre[:, k:k + 1], mg=magb:
                              e.tensor_tensor_scan(out=o, data0=mg, data1=d1, initial=ini, op0=ALU.mult, op1=ALU.add),
                              reads=[self.tok(p(f"vv{q}0")), self.tok(S5), self.tok(PT)], writes=[self.tok(p(f"ss{q}0"))])
                    self.P.op("dve", lambda e, o=ss[q][1][:], d1=vv[q][1][:], ini=sim[:, k:k + 1], mg=magb:
                              e.tensor_tensor_scan(out=o, data0=mg, data1=d1, initial=ini, op0=ALU.mult, op1=ALU.add),
                              reads=[self.tok(p(f"vv{q}1")), self.tok(S5), self.tok(PT)], writes=[self.tok(p(f"ss{q}1"))])
                    self.act(lre[:, k:k + 1], ss[q][0][:, T - 1:T], AF.Copy, [p(f"ss{q}0")], [p("lst")])
                    self.act(lim[:, k:k + 1], ss[q][1][:, T - 1:T], AF.Copy, [p(f"ss{q}1")], [p("lst")])

                def st3b(k):
                    q = k % 2
                    j, m = k // 4, k % 4
                    cr = cosT[:, k, :]; sr_ = sinT[:, k, :]
                    self.tt(pp[q][0][:], ss[q][0][:], cr, ALU.mult, [p(f"ss{q}0"), TB], [p(f"pp{q}0")])
                    self.tt(pp[q][1][:], ss[q][1][:], sr_, ALU.mult, [p(f"ss{q}1"), TB], [p(f"pp{q}1")], eng="pool")
                    self.tt(pp[q][2][:], ss[q][0][:], sr_, ALU.mult, [p(f"ss{q}0"), TB], [p(f"pp{q}2")], eng="pool")
                    self.tt(pp[q][3][:], ss[q][1][:], cr, ALU.mult, [p(f"ss{q}1"), TB], [p(f"pp{q}3")],
                            eng=("pool" if k % 2 == 0 else "dve"))
                    ytk = f"ps{6 + j % 2}"
                    yps = PS[6 + j % 2][:, 0:T]
                    self.mm(yps, CT[0][:, k, :], pp[q][0][:], m == 0, False, [p("CT"), p(f"pp{q}0")], [ytk], sig=False)
                    self.mm(yps, CT[1][:, k, :], pp[q][1][:], False, False, [p("CT"), p(f"pp{q}1")], [ytk], sig=False)
                    self.mm(yps, CT[2][:, k, :], pp[q][2][:], False, False, [p("CT"), p(f"pp{q}2")], [ytk], sig=False)
                    self.mm(yps, CT[2][:, k, :], pp[q][3][:], False, m == 3, [p("CT"), p(f"pp{q}3")], [ytk])
                    if m == 3:
                        self.stt(yj[:], uT[:, j, :], dsk[:, j:j + 1], yps, ALU.mult, ALU.add, [p("uT"), p("dsk"), ytk], [p("yj")])
                        self.act(g1[:], yj[:], AF.Square, [p("yj")], [p("g1")])
                        self.ts(g1[:], g1[:], 0.044715, 1.0, ALU.mult, ALU.add, [p("g1")], [p("g1")])
                        self.tt(g1[:], g1[:], yj[:], ALU.mult, [p("g1"), p("yj")], [p("g1")])
                        self.act(g2[:], g1[:], AF.Sigmoid, [p("g1")], [p("g2")], scale=1.5957691216057308)
                        self.tt(yg[:, j, :], yj[:], g2[:], ALU.mult, [p("yj"), p("g2")], [p("yg")])
                        self.cp(ygb[:, j, :], yg[:, j, :], [p("yg")], [p("ygb")], eng="pool")

                if self.cut >= 5:
                    st1(0)
                    for k in range(16):
                        if k + 1 < 16:
                            st1(k + 1)
                        st2(k)
                        if k >= 1:
                            st3a(k - 1)
                            st3b(k - 1)
                    st3a(15)
                    st3b(15)
                if self.cut < 5:
                    self.emit_E(t, t0, xs, mixT, Wout, xout, p)
                    continue
                cT_ = cosT[:, :, T - 1]; sT_ = sinT[:, :, T - 1]
                self.tt(ctmp[:, 0, :], lre[:], cT_, ALU.mult, [p("lst"), TB], [p("ctmp")])
                self.tt(ctmp[:, 1, :], lim[:], sT_, ALU.mult, [p("lst"), TB], [p("ctmp")])
                self.tt(ctmp[:, 2, :], lre[:], sT_, ALU.mult, [p("lst"), TB], [p("ctmp")])
                self.tt(ctmp[:, 3, :], lim[:], cT_, ALU.mult, [p("lst"), TB], [p("ctmp")])
                self.tt(sre[:], ctmp[:, 0, :], ctmp[:, 1, :], ALU.subtract, [p("ctmp")], [S5])
                self.tt(sim[:], ctmp[:, 2, :], ctmp[:, 3, :], ALU.add, [p("ctmp")], [S5])
                Y2 = [p(f"y2{n}") for n in range(4)]
                SQ = [p(f"sq{n}") for n in range(4)]
                for n in range(4):
                    zp = PS[2 + n][:, 0:T]
                    for j in range(4):
                        self.mm(zp, Wglu[:, j, n * 128:(n + 1) * 128], ygb[:, j, :], j == 0, j == 3,
                                [p("Wglu"), p("ygb")], [f"ps{2 + n}"], sig=(j == 3))
                for n in range(4):
                    self.act(y2[:, n, :], PS[2 + n][:, 0:T], AF.Sigmoid, [f"ps{2 + n}", p("bglu")], [Y2[n]],
                             bias=bglu[:, n:n + 1])
                for n in range(4):
                    self.tt(y2[:, n, :], yg[:, n, :], y2[:, n, :], ALU.mult, [p("yg"), Y2[n]], [Y2[n]])
                for n in range(4):
                    self.act(sq[:, n, :], y2[:, n, :], AF.Square, [Y2[n]], [SQ[n]])
                msp = PS[6][:, 0:T]
                for n in range(4):
                    self.mm(msp, self.avg512[:], sq[:, n, :], n == 0, n == 3, ["avg512", SQ[n]], ["ps6"], sig=(n == 3))
                self.ts(msp, msp, 1.0, EPS, ALU.mult, ALU.add, ["ps6"], ["ps6"])
                self.act(msp, msp, AF.Sqrt, ["ps6"], ["ps6"])
                self.P.op("dve", lambda e, o=msp: e.reciprocal(out=o, in_=o), reads=[self.tok("ps6")], writes=[self.tok("ps6")])
                for n in range(4):
                    self.stt(mixT[:, n, :], y2[:, n, :], gs5[:, n:n + 1], msp, ALU.mult, ALU.mult,
                             [Y2[n], p("gs5"), "ps6"], [p(f"mixT{n}")])
                if self.cut < 5.1:
                    self.emit_E(t, t0, xs, mixT, Wout, xout, p)
                    continue
                LT = [Y2[0], Y2[1]]
                for b in range(2):
                    bc = slice(b * 128, (b + 1) * 128)
                    zb = PS[7 - b]; ztk = f"ps{7 - b}"
                    zp = zb[:, 0:256]
                    self.mm(zp, glT[:, bc], Wa2[:], True, False, [p("glT"), p("Wa2")], [ztk], sig=False)
                    self.mm(zp, self.ones1[:], ba2[:], False, True, ["ones1", p("ba2")], [ztk])
                    self.act(ltm[:, b, :], zp, AF.Exp, [ztk], [LT[b]], scale=-1.0)
                    self.act(ltm[:, b, :], ltm[:, b, :], AF.Ln, [LT[b]], [LT[b]], bias=1.0)
                    for hp in range(2):
                        cb = PS[2 + 2 * b + hp]; ctk = f"ps{2 + 2 * b + hp}"
                        self.mm(cb[:, 0:128], ltm[:, b, hp * 128:(hp + 1) * 128], self.triM[:], True, True,
                                [LT[b], "triM"], [ctk])
                    rp = zb[:, 256:512]
                    self.mm(rp, self.triU[:], ltm[:, b, :], True, True, ["triU", LT[b]], [ztk])
                    for hp in range(2):
                        cb = PS[2 + 2 * b + hp]; ctk = f"ps{2 + 2 * b + hp}"
                        self.act(cb[:, 128:256], cb[:, 0:128], AF.Exp, [ctk], [ctk])
                        self.act(cb[:, 256:384], cb[:, 0:128], AF.Exp, [ctk], [ctk], scale=-1.0)
                    self.act(rp, rp, AF.Exp, [ztk], [ztk])
                    for hp in range(2):
                        cb = PS[2 + 2 * b + hp]; ctk = f"ps{2 + 2 * b + hp}"
                        Eq = cb[:, 128:256]; Ek = cb[:, 256:384]
                        self.ts(dec[:, hp, 2 * b:2 * b + 2], Eq.rearrange("p (c i) -> p c i", c=2)[:, :, 63], 1.0, None,
                                ALU.mult, None, [ctk], [p("dec")])
                        self.stt(qtT[:, hp, bc], qkT[:, hp, bc], 0.125, Eq, ALU.mult, ALU.mult,
                                 [p("qkT"), ctk], [p("qtT")])
                        for i in range(2):
                            hs = slice(i * 64, (i + 1) * 64)
                            self.tt(ktTz[i][hs, hp, bc], qkT[hs, 2 + hp, bc], Ek[hs, :], ALU.mult,
                                    [p("qkT"), ctk], [p("ktT")])
                    for i in range(2):
                        hs = slice(i * 64, (i + 1) * 64)
                        self.tt(kendz[i][hs, b, :], ktm[hs, b, :], rp[hs, :], ALU.mult,
                                [p("ktm"), ztk], [p("kend")])
                oP = [PS[6], PS[7]]
                if self.cut < 5.2:
                    self.emit_E(t, t0, xs, mixT, Wout, xout, p)
                    continue
                for b in range(2):
                    bc0 = b * 128
                    sbk = 2 + 2 * b; stk = f"ps{sbk}"
                    ubk = [3 + 2 * b, 2 + 2 * b]
                    for h in range(4):
                        hp = h // 2
                        self.mm(PS[sbk][:, h * 128:(h + 1) * 128], ktTz[h % 2][:, hp, bc0:bc0 + 128],
                                qtT[:, hp, bc0:bc0 + 128], True, True, [p("ktT"), p("qtT")], [stk], sig=(h == 3))
                    self.tt(scTh[:, :, :], PS[sbk][:, :].rearrange("p (h i) -> p h i", h=4),
                            self.maskC[:].unsqueeze(1).to_broadcast([128, 4, 128]), ALU.mult, [stk, "maskC"], [p("scTh")])
                    for c in range(2):
                        g = 2 * b + c
                        c0 = bc0 + c * 64
                        utk = f"ps{ubk[c]}"
                        for h in range(4):
                            hp = h // 2
                            self.mm(PS[ubk[c]][:, h * 128:(h + 1) * 128],
                                    kendz[c][:, b, hp * 128:(hp + 1) * 128],
                                    vtm[:, b, h * 128:(h + 1) * 128], True, True,
                                    [p("kend"), p("vtm")], [utk], sig=(h == 3))
                        rin = (g + 1) % 2
                        for h in range(4):
                            hp = h // 2
                            ob = oP[hp][:, (h % 2) * 256 + c0:(h % 2) * 256 + c0 + 64]
                            otk = f"ps{6 + hp}"
                            self.mm(ob, Sgbz[rin][h % 2][:, hp, :], qtT[:, hp, c0:c0 + 64], True, False,
                                    [p(f"Sgb{rin}"), p("qtT")], [otk], sig=False)
                            self.mm(ob, vtm[:, b, h * 128:(h + 1) * 128],
                                    scTh[:, h, c * 64:(c + 1) * 64], False, True,
                                    [p("vtm"), p("scTh")], [otk])
                        for h in range(4):
                            hp, po = h // 2, (h % 2) * 64
                            hs = slice(po, po + 64)
                            self.stt(Sg[hs, hp, :], Sg[hs, hp, :], dec[hs, hp, g:g + 1],
                                     PS[ubk[c]][hs, h * 128:(h + 1) * 128], ALU.mult, ALU.add,
                                     [p("Sg"), p("dec"), utk], [p("Sg")])
                        for i in range(2):
                            hs = slice(i * 64, (i + 1) * 64)
                            self.cp(Sgbz[g % 2][i][hs, :, :], Sg[hs, :, :], [p("Sg")], [p(f"Sgb{g % 2}")], eng="act")
                if self.cut < 5.5:
                    self.emit_E(t, t0, xs, mixT, Wout, xout, p)
                    continue
                for hp in range(2):
                    self.act(o32[:, 2 * hp:2 * hp + 2, :], oP[hp][:, :].rearrange("p (h t) -> p h t", h=2), AF.Copy,
                             [f"ps{6 + hp}"], [p("yg")])
                self.act(sq[:], o32[:], AF.Square, [p("yg")], SQ)
                for h in range(4):
                    self.mm(PS[2 + h][:, 0:256], self.avg128[:], sq[:, h, :], True, True, ["avg128", SQ[h]], [f"ps{2 + h}"])
                for h in range(4):
                    pb = PS[2 + h][:, 0:256]
                    self.ts(pb, pb, 1.0, EPS, ALU.mult, ALU.add, [f"ps{2 + h}"], [f"ps{2 + h}"])
                for h in range(4):
                    pb = PS[2 + h][:, 0:256]
                    self.act(pb, pb, AF.Sqrt, [f"ps{2 + h}"], [f"ps{2 + h}"])
                for h in range(4):
                    pb = PS[2 + h][:, 0:256]
                    self.P.op("dve", lambda e, o=pb: e.reciprocal(out=o, in_=o), reads=[self.tok(f"ps{2 + h}")],
                              writes=[self.tok(f"ps{2 + h}")])
                for h in range(4):
                    pb = PS[2 + h][:, 0:256]
                    self.stt(pb, o32[:, h, :], ggla[:, h:h + 1], pb, ALU.mult, ALU.mult,
                             [p("yg"), p("ggla"), f"ps{2 + h}"], [f"ps{2 + h}"])
                for h in range(4):
                    pb = PS[2 + h][:, 0:256]
                    self.tt(mixT[:, 4 + h, :], pb, srT[:, h, :], ALU.mult, [f"ps{2 + h}", p("srT")], [p(f"mixT{4 + h}")])
                self.emit_E(t, t0, xs, mixT, Wout, xout, p)

    def emit_E(self, t, t0, xs, mixT, Wout, xout, p):
        PS = self.psum
        for b in range(2):
            slot = (2 * t + b) % 4
            xtok = p(f"xs{slot}")
            for half in range(2):
                pb = PS[half]
                for k in range(8):
                    self.mm(pb[:, :], mixT[:, k, b * 128:(b + 1) * 128], Wout[:, k, half * 512:(half + 1) * 512],
                            k == 0, k == 7, [p(f"mixT{k}"), p("Wout")], [f"ps{half}"], sig=(k == 7))
                self.tt(xs[slot][:, half * 512:(half + 1) * 512], pb[:, :], xs[slot][:, half * 512:(half + 1) * 512],
                        ALU.add, [f"ps{half}", xtok], [xtok])
            key = ("yout" if xout.tensor.name == "y" else p("st")) + str(b)
            self.dma("pool", xout[t0 + b * 128:t0 + (b + 1) * 128, :], xs[slot][:], key, [xtok],
                     [f"dram_{xout.tensor.name}"])

    def ffn(self, l, xin, xout, final):
        nc, dr, L = self.nc, self.dr, self.L
        pfx = f"f{l}_"
        p = lambda s: pfx + s
        PS = self.psum
        moe = (l == 1)
        ST = min(L, 2048)
        NST = L // ST
        NB = ST // 128
        NTL = ST // 256
        if moe:
            experts = list(range(NE)); FC = D_FFE // 128
        else:
            experts = [0]; FC = D_FF // 128
        slices = []
        for e in experts:
            c = 0
            while c < FC:
                n = min(4, FC - c)
                slices.append((e, c, n))
                c += n
        self.fence()
        with contextlib.ExitStack() as sk:
            sb = lambda name, shape, dt=F32: self.sb(sk, pfx + name, shape, dt)
            xacc = sb("xacc", [128, NB, D])
            hT = sb("hT", [128, 8, ST], BF16)
            gffn = sb("gffn", [128, 8]); self.load_colvec(gffn[:], dr["norm_ffn"][l], p("gffn"), p("gffn"))
            scr = {"stat": sb("stat", [128, 4]), "junk": sb("junk", [128, D], BF16), "xn": sb("xn", [128, D])}
            Wg = [sb(f"Wg{i}", [128, 8, 512], BF16) for i in range(2)]
            Wu = [sb(f"Wu{i}", [128, 8, 512], BF16) for i in range(2)]
            Wd = [sb(f"Wd{i}", [128, 4, D], BF16) for i in range(2)]
            sg = [sb(f"sg{i}", [128, 256]) for i in range(3)]
            aT = [sb(f"aT{i}", [128, 256], BF16) for i in range(6)]
            gates = sb("gates", [128, NB, NE])
            if moe:
                h32 = sb("h32", [128, 8, 128])
                Wr = sb("Wr", [128, 8, NE])
                self.dma("sync", Wr[:], dr["moe_w_router"][0].rearrange("(k q) e -> q k e", q=128), p("Wr"), [], [p("Wr")])
                lg = sb("lg", [128, 8]); mx = sb("mx", [128, 8]); rt = sb("rt", [128, 8]); gt = sb("gt", [128, 8])
            if final:
                gfin = sb("gfin", [128, D])
                self.dma("sync", gfin[:], dr["norm_final"].partition_broadcast(128), p("gfin"), [], [p("gfin")])
                yo = [sb(f"yo{i}", [128, D]) for i in range(2)]

            def wsrc(kind, e):
                if moe:
                    nm = {"g": "moe_w_gate", "u": "moe_w_up", "d": "moe_w_down"}[kind]
                    return dr[nm][0, e]
                nm = {"g": "ffn_w_gate", "u": "ffn_w_up", "d": "ffn_w_down"}[kind]
                return dr[nm][0]

            def load_slice(si, slot):
                e, c, n = slices[si]
                f0 = c * 128
                self.dma("pool", Wg[slot][:, :, 0:n * 128],
                         wsrc("g", e).rearrange("(k q) f -> q k f", q=128)[:, :, f0:f0 + n * 128],
                         p(f"Wg{slot}"), [], [p(f"Wg{slot}")])
                self.dma("pool", Wu[slot][:, :, 0:n * 128],
                         wsrc("u", e).rearrange("(k q) f -> q k f", q=128)[:, :, f0:f0 + n * 128],
                         p(f"Wu{slot}"), [], [p(f"Wu{slot}")])
                self.dma("pool", Wd[slot][:, 0:n, :],
                         wsrc("d", e)[f0:f0 + n * 128, :].rearrange("(c q) d -> q c d", q=128),
                         p(f"Wd{slot}"), [], [p(f"Wd{slot}")])

            for s in range(NST):
                r0 = s * ST
                load_slice(0, 0)
                for b in range(NB):
                    xtok = p(f"xacc{b}")
                    self.dma("sync", xacc[:, b, :], xin[r0 + b * 128:r0 + (b + 1) * 128, :], p(f"xl{b % 4}"),
                             [f"dram_{xin.tensor.name}"], [xtok])
                    self.norm_block(sk, pfx, xacc[:, b, :], xtok, gffn, p("gffn"),
                                    hT[:, :, b * 128:(b + 1) * 128], p("hT"), [PS[6], PS[7]], ["ps6", "ps7"], scr,
                                    h32_dst=(h32 if moe else None))
                    if moe:
                        lp = PS[5][:, 0:NE]
                        for k in range(8):
                            self.mm(lp, h32[:, k, :], Wr[:, k, :], k == 0, k == 7, [p("hT32"), p("Wr")], ["ps5"], sig=(k == 7))
                        G = p("gt")
                        self.cp(lg[:], lp, ["ps5"], [G])
                        self.P.op("dve", lambda e: e.max(out=mx[:], in_=lg[:]), reads=[self.tok(G)], writes=[self.tok(G)])
                        self.tt(rt[:, 0:1], mx[:, 1:2], mx[:, 0:1], ALU.subtract, [G], [G])
                        self.act(rt[:, 1:2], rt[:, 0:1], AF.Exp, [G], [G])
                        self.ts(rt[:, 2:3], rt[:, 1:2], 1.0, None, ALU.add, None, [G], [G])
                        self.P.op("dve", lambda e: e.reciprocal(out=rt[:, 3:4], in_=rt[:, 2:3]), reads=[self.tok(G)], writes=[self.tok(G)])
                        self.tt(rt[:, 4:5], rt[:, 1:2], rt[:, 3:4], ALU.mult, [G], [G])
                        self.ts(gt[:], lg[:], mx[:, 0:1], rt[:, 3:4], ALU.is_equal, ALU.mult, [G], [G])
                        self.ts(lg[:], lg[:], mx[:, 1:2], rt[:, 4:5], ALU.is_equal, ALU.mult, [G], [G])
                        self.tt(gates[:, b, :], gt[:], lg[:], ALU.add, [G], [p("gates")])
                units = [(si, tl, cc) for si, (e_, c_, n_) in enumerate(slices) for tl in range(NTL) for cc in range(n_)]
                NU = len(units)
                loaded = {0}

                def emit_gu(u):
                    si, tl, cc = units[u]
                    e, c, n = slices[si]
                    slot = si % 2
                    WT = [p(f"Wg{slot}"), p(f"Wu{slot}"), p(f"Wd{slot}")]
                    tc = slice(tl * 256, (tl + 1) * 256)
                    gb = PS[4 + u % 4]
                    gtk = f"ps{4 + u % 4}"
                    gp = gb[:, 0:256]; up = gb[:, 256:512]
                    for k in range(8):
                        self.mm(gp, Wg[slot][:, k, cc * 128:(cc + 1) * 128], hT[:, k, tc], k == 0, k == 7,
                                [p("hT"), WT[0]], [gtk], sig=False)
                    for k in range(8):
                        self.mm(up, Wu[slot][:, k, cc * 128:(cc + 1) * 128], hT[:, k, tc], k == 0, k == 7,
                                [p("hT"), WT[1]], [gtk], sig=(k == 7))
                    sgi = sg[u % 3]
                    self.act(sgi[:], gp, AF.Silu, [gtk], [p(f"sg{u % 3}")])
                    self.tt(aT[u % 6][:], sgi[:], up, ALU.mult, [p(f"sg{u % 3}"), gtk], [p(f"aT{u % 6}")])

                def emit_down(u):
                    si, tl, cc = units[u]
                    e, c, n = slices[si]
                    slot = si % 2
                    if si + 1 < len(slices) and (si + 1) not in loaded:
                        loaded.add(si + 1)
                        load_slice(si + 1, 1 - slot)
                    for b2 in range(2):
                        for half in range(2):
                            ai = b2 * 2 + half
                            self.mm(PS[ai][:, :], aT[u % 6][:, b2 * 128:(b2 + 1) * 128],
                                    Wd[slot][:, cc, half * 512:(half + 1) * 512], cc == 0, cc == n - 1,
                                    [p(f"aT{u % 6}"), p(f"Wd{slot}")], self.pst(ai), sig=(cc == n - 1))
                    if cc == n - 1:
                        for b2 in range(2):
                            b = tl * 2 + b2
                            xtok = p(f"xacc{b}")
                            for half in range(2):
                                ai = b2 * 2 + half
                                dst = xacc[:, b, half * 512:(half + 1) * 512]
                                if moe:
                                    self.stt(dst, PS[ai][:, :], gates[:, b, e:e + 1], dst, ALU.mult, ALU.add,
                                             self.pst(ai) + [p("gates"), xtok], [xtok])
                                else:
                                    self.tt(dst, PS[ai][:, :], dst, ALU.add, self.pst(ai) + [xtok], [xtok])

                for u in range(min(3, NU)):
                    emit_gu(u)
                for u in range(NU):
                    if u + 3 < NU:
                        emit_gu(u + 3)
                    emit_down(u)
                for b in range(NB):
                    xtok = p(f"xacc{b}")
                    rows = slice(r0 + b * 128, r0 + (b + 1) * 128)
                    if final:
                        o = b % 2
                        stt_ = scr["stat"]
                        self.act(scr["junk"][:], xacc[:, b, :], AF.Square, [xtok], [p("junk"), p("stat")], accum=stt_[:, 0:1])
                        self.rsqrt_(stt_[:, 1:2], stt_[:, 0:1], stt_[:, 2:3], [p("stat")], [p("stat")], mul=1.0 / D)
                        self.stt(yo[o][:], xacc[:, b, :], stt_[:, 1:2], gfin[:], ALU.mult, ALU.mult,
                                 [xtok, p("stat"), p("gfin")], [p(f"yo{o}")])
                        self.dma("sync", xout[rows, :], yo[o][:], f"yout{o}", [p(f"yo{o}")], [f"dram_{xout.tensor.name}"])
                    else:
                        key = ("yout" if xout.tensor.name == "y" else p("st")) + str(b % 2)
                        self.dma("sync", xout[rows, :], xacc[:, b, :], key, [xtok], [f"dram_{xout.tensor.name}"])


_CACHE = {}


def get_nc(L, stop_after=None):
    key = (L, stop_after)
    if key not in _CACHE:
        _CACHE[key] = K(L, stop_after).build()
    return _CACHE[key]


NAMES = ["norm_mix", "w_in", "s5_lambda_re", "s5_lambda_im", "s5_log_dt", "s5_b_re", "s5_b_im", "s5_c_re", "s5_c_im",
         "s5_d", "s5_w_glu", "s5_b_glu", "s5_out_norm", "gla_w_a2", "gla_b_a2", "gla_out_norm", "w_out", "norm_ffn",
         "ffn_w_gate", "ffn_w_up", "ffn_w_down", "moe_w_router", "moe_w_gate", "moe_w_up", "moe_w_down", "norm_final"]


def kernel(**inputs):
    x = np.ascontiguousarray(np.asarray(inputs["x"], dtype=np.float32))
    B, L, _ = x.shape
    nc = get_nc(L)
    shared = {n: np.ascontiguousarray(np.asarray(inputs[n], dtype=np.float32)) for n in NAMES}
    in_maps = []
    for b in range(B):
        m = dict(shared)
        m["x"] = x[b]
        in_maps.append(m)
    res = run_bass_kernel_spmd(nc, in_maps, core_ids=list(range(B)))
    return np.stack([np.asarray(r["y"], dtype=np.float32) for r in res.results], axis=0)
```

```python
import contextlib
import math
import numpy as np
import concourse.bass as bass
import concourse.mybir as mybir
from concourse.bass_utils import run_bass_kernel_spmd

F32 = mybir.dt.float32
BF16 = mybir.dt.bfloat16
I32 = mybir.dt.int32
AF = mybir.ActivationFunctionType
ALU = mybir.AluOpType

D = 1024
D_IN = 2064
D_FF = 2816
D_FFE = 3584
NE = 8
EPS = 1e-6
TWO_PI = 2.0 * math.pi

COMPUTE = ("pe", "act", "dve", "pool")
NO_SELF_WAIT = ()


class Tok:
    __slots__ = ("name", "w", "r")

    def __init__(self, name):
        self.name = name
        self.w = None
        self.r = {}


class Prog:
    def __init__(self, nc):
        self.nc = nc
        self.ops = {e: [] for e in ("pe", "act", "dve", "pool", "sync")}
        self.seq = {e: 0 for e in COMPUTE}
        self.waited = {e: {} for e in self.ops}
        self.dma_cnt = {}
        self.semkeys = []
        self.pending_sig = {e: False for e in COMPUTE}

    def _semkey(self, k):
        if k not in self.semkeys:
            self.semkeys.append(k)
        return k

    def _deps(self, reads, writes):
        deps = {}

        def add(k, v):
            if deps.get(k, 0) < v:
                deps[k] = v
        for t in reads:
            if t.w is not None:
                add(*t.w)
        for t in writes:
            if t.w is not None:
                add(*t.w)
            for k, v in t.r.items():
                add(k, v)
        return deps

    def _emit_waits(self, eng, deps):
        for k, v in deps.items():
            if k == eng and (eng == "pe" or eng in NO_SELF_WAIT):
                continue
            if self.waited[eng].get(k, 0) >= v:
                continue
            self.waited[eng][k] = v
            self.ops[eng].append(("wait", k, v))

    def op(self, eng, fn, reads=(), writes=(), sig=True):
        deps = self._deps(reads, writes)
        self._emit_waits(eng, deps)
        if sig:
            self.seq[eng] += 1
            comp = (eng, self.seq[eng])
            self.pending_sig[eng] = False
        else:
            comp = (eng, self.seq[eng] + 1)
            self.pending_sig[eng] = True
        self._semkey(eng)
        self.ops[eng].append(("op", fn, sig, eng))
        for t in writes:
            t.w = comp
            t.r = {}
        for t in reads:
            if t.r.get(comp[0], 0) < comp[1]:
                t.r[comp[0]] = comp[1]

    def frontier(self):
        f = {e: self.seq[e] + (1 if self.pending_sig[e] else 0) for e in COMPUTE if self.seq[e] or self.pending_sig[e]}
        f.update(self.dma_cnt)
        return f

    def dma(self, q, fn, key, reads=(), writes=()):
        deps = self._deps(reads, writes)
        k = self._semkey("dma_" + key)
        if self.dma_cnt.get(k, 0) > 0 and deps.get(k, 0) < self.dma_cnt[k]:
            deps[k] = self.dma_cnt[k]
        self._emit_waits(q, deps)
        self.dma_cnt[k] = self.dma_cnt.get(k, 0) + 16
        comp = (k, self.dma_cnt[k])
        self.ops[q].append(("dma", fn, k))
        for t in writes:
            t.w = comp
            t.r = {}
        for t in reads:
            if t.r.get(k, 0) < comp[1]:
                t.r[k] = comp[1]
        return comp

    def finish(self, final_waits):
        nc = self.nc
        for e in COMPUTE:
            assert not self.pending_sig[e], f"pending unsignalled op on {e}"
        with contextlib.ExitStack() as st:
            sems = {}
            for k in self.semkeys:
                sems[k] = st.enter_context(nc.semaphore(k))
            block = st.enter_context(nc.Block())

            def replay(ename, eng_obj, extra=()):
                for it in self.ops[ename]:
                    if it[0] == "wait":
                        eng_obj.wait_ge(sems[it[1]], it[2])
                    elif it[0] == "op":
                        ins = it[1](eng_obj)
                        if it[2]:
                            ins.then_inc(sems[it[3]], 1)
                    else:
                        ins = it[1](eng_obj)
                        ins.then_inc(sems[it[2]], 16)
                for k, v in extra:
                    eng_obj.wait_ge(sems[k], v)

            @block.tensor
            def _(e):
                replay("pe", e)

            @block.scalar
            def _(e):
                replay("act", e)

            @block.vector
            def _(e):
                replay("dve", e)

            @block.gpsimd
            def _(e):
                replay("pool", e, extra=final_waits)

            @block.sync
            def _(e):
                replay("sync", e)


class K:
    def __init__(self, L, stop_after=None, cut=99):
        self.L = L
        self.cut = cut
        self.stop_after = stop_after
        self.nc = bass.Bass("TRN2", target_bir_lowering=False)
        self.P = Prog(self.nc)
        self.toks = {}
        self.base = {}

    def tok(self, name):
        if name not in self.toks:
            t = Tok(name)
            t.r = dict(self.base)
            self.toks[name] = t
        return self.toks[name]

    def fence(self):
        self.base = self.P.frontier()

    def pst(self, i):
        return [f"ps{i}"]

    def mm(self, out, lhsT, rhs, start, stop, r, w, sig=True):
        self.P.op("pe", lambda e: e.matmul(out, lhsT, rhs, start=start, stop=stop),
                  reads=[self.tok(x) for x in r], writes=[self.tok(x) for x in w], sig=sig)

    def tr(self, out, in_, ident, r, w, sig=True):
        self.P.op("pe", lambda e: e.transpose(out, in_, ident),
                  reads=[self.tok(x) for x in r], writes=[self.tok(x) for x in w], sig=sig)

    def act(self, out, in_, func, r, w, bias=None, scale=None, accum=None):
        kw = {}
        if bias is not None:
            kw["bias"] = bias
        if scale is not None:
            kw["scale"] = scale
        if accum is not None:
            kw["accum_out"] = accum
        self.P.op("act", lambda e: e.activation(out=out, in_=in_, func=func, **kw),
                  reads=[self.tok(x) for x in r], writes=[self.tok(x) for x in w])

    def ts(self, out, in0, s1, s2, op0, op1, r, w, eng="dve"):
        if op1 is None:
            f = lambda e: e.tensor_scalar(out=out, in0=in0, scalar1=s1, scalar2=None, op0=op0)
        else:
            f = lambda e: e.tensor_scalar(out=out, in0=in0, scalar1=s1, scalar2=s2, op0=op0, op1=op1)
        self.P.op(eng, f, reads=[self.tok(x) for x in r], writes=[self.tok(x) for x in w])

    def stt(self, out, in0, scalar, in1, op0, op1, r, w):
        self.P.op("dve", lambda e: e.scalar_tensor_tensor(out=out, in0=in0, scalar=scalar, in1=in1,
                                                          op0=op0, op1=op1),
                  reads=[self.tok(x) for x in r], writes=[self.tok(x) for x in w])

    def tt(self, out, in0, in1, op, r, w, eng="dve"):
        self.P.op(eng, lambda e: e.tensor_tensor(out=out, in0=in0, in1=in1, op=op),
                  reads=[self.tok(x) for x in r], writes=[self.tok(x) for x in w])

    def cp(self, out, in_, r, w, eng="dve"):
        if eng == "act":
            return self.act(out, in_, AF.Copy, r, w)
        if eng == "dve" and any(x.startswith("ps") for x in r):
            return self.ts(out, in_, 1.0, None, ALU.mult, None, r, w)
        self.P.op(eng, lambda e: e.tensor_copy(out=out, in_=in_),
                  reads=[self.tok(x) for x in r], writes=[self.tok(x) for x in w])

    def memset(self, ap, val, w, eng="pool"):
        self.P.op(eng, lambda e: e.memset(ap, val), writes=[self.tok(x) for x in w])

    def dma(self, q, out, in_, key, r, w, **kw):
        return self.P.dma(q, lambda e: e.dma_start(out=out, in_=in_, **kw), key,
                          reads=[self.tok(x) for x in r], writes=[self.tok(x) for x in w])

    def rsqrt_(self, out, in_, tmp, r, w, eps=EPS, mul=1.0):
        self.ts(tmp, in_, mul, eps, ALU.mult, ALU.add, r, w)
        self.act(tmp, tmp, AF.Sqrt, w, w)
        self.P.op("dve", lambda e: e.reciprocal(out=out, in_=tmp),
                  reads=[self.tok(x) for x in w], writes=[self.tok(x) for x in w])

    def build(self):
        nc = self.nc
        L = self.L
        dr = {}

        def din(name, shape):
            dr[name] = nc.dram_tensor(name, list(shape), F32, kind="ExternalInput").ap()
        din("x", [L, D])
        din("norm_mix", [2, D]); din("w_in", [2, D, D_IN])
        din("s5_lambda_re", [2, 32, 64]); din("s5_lambda_im", [2, 32, 64]); din("s5_log_dt", [2, 32])
        din("s5_b_re", [2, 32, 64, 16]); din("s5_b_im", [2, 32, 64, 16])
        din("s5_c_re", [2, 32, 16, 64]); din("s5_c_im", [2, 32, 16, 64])
        din("s5_d", [2, 512]); din("s5_w_glu", [2, 512, 512]); din("s5_b_glu", [2, 512])
        din("s5_out_norm", [2, 512]); din("gla_w_a2", [2, 16, 256]); din("gla_b_a2", [2, 256])
        din("gla_out_norm", [2, 512]); din("w_out", [2, D, D]); din("norm_ffn", [2, D])
        din("ffn_w_gate", [1, D, D_FF]); din("ffn_w_up", [1, D, D_FF]); din("ffn_w_down", [1, D_FF, D])
        din("moe_w_router", [1, D, NE]); din("moe_w_gate", [1, NE, D, D_FFE])
        din("moe_w_up", [1, NE, D, D_FFE]); din("moe_w_down", [1, NE, D_FFE, D])
        din("norm_final", [D])
        self.dr = dr
        y = nc.dram_tensor("y", [L, D], F32, kind="ExternalOutput").ap()
        s1 = nc.dram_tensor("scr1", [L, D], F32, kind="Internal").ap()
        s2 = nc.dram_tensor("scr2", [L, D], F32, kind="Internal").ap()
        sa = self.stop_after
        with contextlib.ExitStack() as st:
            self.st = st
            self.psum = [st.enter_context(nc.psum_tensor(f"ps{i}", [128, 512], F32)) for i in range(8)]
            self.consts()
            if sa == "mix0":
                self.mixer(0, dr["x"], y)
            elif sa == "ffn0":
                self.mixer(0, dr["x"], s1)
                self.ffn(0, s1, y, final=False)
            elif sa == "mix1":
                self.mixer(0, dr["x"], s1)
                self.ffn(0, s1, s2, final=False)
                self.mixer(1, s2, y)
            elif sa == "ffnonly":
                self.ffn(0, dr["x"], y, final=False)
            elif sa == "moeonly":
                self.ffn(1, dr["x"], y, final=True)
            else:
                self.mixer(0, dr["x"], s1)
                self.ffn(0, s1, s2, final=False)
                self.mixer(1, s2, s1)
                self.ffn(1, s1, y, final=True)
            fw = [(k, v) for k, v in self.P.dma_cnt.items() if k.startswith("dma_yout")]
            self.P.finish(fw)
        return nc

    def sb(self, stack, name, shape, dt=F32):
        return stack.enter_context(self.nc.sbuf_tensor(name, list(shape), dt))

    def consts(self):
        st = self.st
        self.ident = self.sb(st, "ident", [128, 128])
        self.onesf = self.sb(st, "onesf", [128, 128])
        self.memset(self.onesf[:], 1.0, ["onesf"])
        self.P.op("pool", lambda e: e.affine_select(out=self.ident[:], in_=self.onesf[:], pattern=[[-1, 128]],
                                                    compare_op=ALU.is_equal, fill=0.0, base=0,
                                                    channel_multiplier=1),
                  reads=[self.tok("onesf")], writes=[self.tok("ident")])
        self.triM = self.sb(st, "triM", [128, 128])
        self.triU = self.sb(st, "triU", [128, 128])
        self.maskC = self.sb(st, "maskC", [128, 128])
        self.blk = self.sb(st, "blkm", [128, 128])
        self.memset(self.blk[:], 0.0, ["blkm"])
        self.memset(self.blk[0:64, 0:64], 1.0, ["blkm"])
        self.memset(self.blk[64:128, 64:128], 1.0, ["blkm"])
        self.P.op("pool", lambda e: e.affine_select(out=self.maskC[:], in_=self.blk[:], pattern=[[1, 128]],
                                                    compare_op=ALU.is_ge, fill=0.0, base=0,
                                                    channel_multiplier=-1),
                  reads=[self.tok("blkm")], writes=[self.tok("maskC")])
        self.ts(self.triM[:], self.maskC[:], -1.0 / 16.0, None, ALU.mult, None, ["maskC"], ["triM"], eng="pool")
        self.P.op("pool", lambda e: e.affine_select(out=self.triU[:], in_=self.blk[:], pattern=[[-1, 128]],
                                                    compare_op=ALU.is_gt, fill=0.0, base=0,
                                                    channel_multiplier=1),
                  reads=[self.tok("blkm")], writes=[self.tok("triU")])
        self.ts(self.triU[:], self.triU[:], -1.0 / 16.0, None, ALU.mult, None, ["triU"], ["triU"], eng="pool")
        self.avg512 = self.sb(st, "avg512", [128, 128], BF16)
        self.avg128 = self.sb(st, "avg128", [128, 128], BF16)
        self.memset(self.avg512[:], 1.0 / 512.0, ["avg512"])
        self.memset(self.avg128[:], 1.0 / 128.0, ["avg128"])
        self.epsc = self.sb(st, "epsc", [128, 1])
        self.memset(self.epsc[:], EPS, ["epsc"])
        self.ones1 = self.sb(st, "ones1", [1, 128])
        self.memset(self.ones1[:], 1.0, ["ones1"])

    def norm_block(self, stk, pfx, xs_ap, xtok, gT, gtok, hT_dst, hT_tok, ps2, pstoks, scr, h32_dst=None):
        stt_ = scr["stat"]
        self.act(scr["junk"][:], xs_ap, AF.Square, [xtok], [pfx + "junk", pfx + "stat"], accum=stt_[:, 0:1])
        self.rsqrt_(stt_[:, 1:2], stt_[:, 0:1], stt_[:, 2:3], [pfx + "stat"], [pfx + "stat"], mul=1.0 / D)
        self.ts(scr["xn"][:], xs_ap, stt_[:, 1:2], None, ALU.mult, None, [xtok, pfx + "stat"], [pfx + "xn"])
        for k in range(8):
            self.tr(ps2[k // 4][:, (k % 4) * 128:(k % 4 + 1) * 128], scr["xn"][:, k * 128:(k + 1) * 128],
                    self.ident[:], [pfx + "xn", "ident"], [pstoks[k // 4]], sig=(k % 4 == 3))
        for hb in range(2):
            self.tt(hT_dst[:, hb * 4:(hb + 1) * 4, :],
                    ps2[hb][:, :].rearrange("p (k t) -> p k t", k=4),
                    gT[:, hb * 4:(hb + 1) * 4].unsqueeze(2).to_broadcast([128, 4, 128]),
                    ALU.mult, [pstoks[hb], gtok], [hT_tok])
            if h32_dst is not None:
                self.tt(h32_dst[:, hb * 4:(hb + 1) * 4, :],
                        ps2[hb][:, :].rearrange("p (k t) -> p k t", k=4),
                        gT[:, hb * 4:(hb + 1) * 4].unsqueeze(2).to_broadcast([128, 4, 128]),
                        ALU.mult, [pstoks[hb], gtok], [hT_tok + "32"])

    def load_colvec(self, dst, src_flat, key, tokn):
        self.dma("sync", dst, src_flat.rearrange("(k q) -> q k", q=128), key, [], [tokn],
                 allow_slow_non_contiguous=True)

    def mixer(self, l, xin, xout):
        nc, dr, L = self.nc, self.dr, self.L
        T = 256
        NT = L // T
        pfx = f"m{l}_"
        PS = self.psum
        self.fence()
        with contextlib.ExitStack() as sk:
            sb = lambda name, shape, dt=F32: self.sb(sk, pfx + name, shape, dt)
            Win = sb("Win", [128, 8, D_IN], BF16)
            for c in range(3):
                self.dma("pool", Win[:, :, c * 688:(c + 1) * 688],
                         dr["w_in"][l].rearrange("(k q) n -> q k n", q=128)[:, :, c * 688:(c + 1) * 688],
                         pfx + f"Win{c}", [], [pfx + f"Win{c}"])
            WIN = [pfx + f"Win{c}" for c in range(3)]
            Wout = sb("Wout", [128, 8, D], BF16)
            self.dma("pool", Wout[:], dr["w_out"][l].rearrange("(k q) n -> q k n", q=128), pfx + "Wout", [], [pfx + "Wout"])
            Wglu = sb("Wglu", [128, 4, 512], BF16)
            self.dma("pool", Wglu[:], dr["s5_w_glu"][l].rearrange("(k q) n -> q k n", q=128), pfx + "Wglu", [], [pfx + "Wglu"])
            Wa2 = sb("Wa2", [16, 256])
            self.dma("sync", Wa2[:], dr["gla_w_a2"][l], pfx + "Wa2", [], [pfx + "Wa2"])
            ba2 = sb("ba2", [1, 256])
            self.dma("sync", ba2[:], dr["gla_b_a2"][l:l + 1, :], pfx + "ba2", [], [pfx + "ba2"])
            gmix = sb("gmix", [128, 8]); self.load_colvec(gmix[:], dr["norm_mix"][l], pfx + "gmix", pfx + "gmix")
            dsk = sb("dsk", [128, 4]); self.load_colvec(dsk[:], dr["s5_d"][l], pfx + "dsk", pfx + "dsk")
            bglu = sb("bglu", [128, 4]); self.load_colvec(bglu[:], dr["s5_b_glu"][l], pfx + "bglu", pfx + "bglu")
            gs5 = sb("gs5", [128, 4]); self.load_colvec(gs5[:], dr["s5_out_norm"][l], pfx + "gs5", pfx + "gs5")
            ggla = sb("ggla", [128, 4]); self.load_colvec(ggla[:], dr["gla_out_norm"][l], pfx + "ggla", pfx + "ggla")
            prm = sb("prm", [128, 24, 16])
            PT = pfx + "prm"

            def pr(i):
                return prm[:, i, :]
            LR, LI, DT, MAG, TH, N_, SN, CS, ABR, ABI, DEN, FRE, FIM, T1, T2, T3 = range(16)
            self.dma("sync", pr(LR), dr["s5_lambda_re"][l].rearrange("(k two) p -> (two p) k", two=2), pfx + "lr", [], [PT],
                     allow_slow_non_contiguous=True)
            self.dma("sync", pr(LI), dr["s5_lambda_im"][l].rearrange("(k two) p -> (two p) k", two=2), pfx + "li", [], [PT],
                     allow_slow_non_contiguous=True)
            for two in range(2):
                self.dma("sync", prm[two * 64:(two + 1) * 64, DT, :],
                         dr["s5_log_dt"][l].rearrange("(k two) -> two k", two=2)[two].partition_broadcast(64),
                         pfx + f"dt{two}", [], [PT], allow_slow_non_contiguous=True)
            self.ts(pr(LR), pr(LR), -1e-4, None, ALU.min, None, [PT], [PT])
            self.act(pr(DT), pr(DT), AF.Exp, [PT], [PT])
            self.tt(pr(T1), pr(LR), pr(DT), ALU.mult, [PT], [PT])
            self.act(pr(MAG), pr(T1), AF.Exp, [PT], [PT])
            self.tt(pr(TH), pr(LI), pr(DT), ALU.mult, [PT], [PT])
            ni = sb("ni", [128, 16], I32)

            def reduce_angle(ap, shape_ni, tmpf, toks):
                self.ts(tmpf, ap, 1.0 / TWO_PI, None, ALU.mult, None, toks, toks)
                self.cp(shape_ni, tmpf, toks, toks)
                self.cp(tmpf, shape_ni, toks, toks)
                self.stt(ap, tmpf, -TWO_PI, ap, ALU.mult, ALU.add, toks, toks)
                self.ts(tmpf, ap, math.pi, -TWO_PI, ALU.is_gt, ALU.mult, toks, toks)
                self.tt(ap, ap, tmpf, ALU.add, toks, toks)
                self.ts(tmpf, ap, -math.pi, TWO_PI, ALU.is_lt, ALU.mult, toks, toks)
                self.tt(ap, ap, tmpf, ALU.add, toks, toks)
            reduce_angle(pr(TH), ni[:], pr(T1), [PT])
            self.act(pr(SN), pr(TH), AF.Sin, [PT], [PT])
            self.ts(pr(T2), pr(TH), math.pi / 2, None, ALU.add, None, [PT], [PT])
            reduce_angle(pr(T2), ni[:], pr(T1), [PT])
            self.act(pr(CS), pr(T2), AF.Sin, [PT], [PT])
            self.tt(pr(ABR), pr(MAG), pr(CS), ALU.mult, [PT], [PT])
            self.tt(pr(ABI), pr(MAG), pr(SN), ALU.mult, [PT], [PT])
            self.ts(pr(ABR), pr(ABR), -1.0, None, ALU.add, None, [PT], [PT])
            self.tt(pr(DEN), pr(LR), pr(LR), ALU.mult, [PT], [PT])
            self.tt(pr(T1), pr(LI), pr(LI), ALU.mult, [PT], [PT])
            self.tt(pr(DEN), pr(DEN), pr(T1), ALU.add, [PT], [PT])
            self.P.op("dve", lambda e: e.reciprocal(out=pr(DEN), in_=pr(DEN)), reads=[self.tok(PT)], writes=[self.tok(PT)])
            self.tt(pr(T1), pr(ABR), pr(LR), ALU.mult, [PT], [PT])
            self.tt(pr(T2), pr(ABI), pr(LI), ALU.mult, [PT], [PT])
            self.tt(pr(T1), pr(T1), pr(T2), ALU.add, [PT], [PT])
            self.tt(pr(FRE), pr(T1), pr(DEN), ALU.mult, [PT], [PT])
            self.tt(pr(T1), pr(ABI), pr(LR), ALU.mult, [PT], [PT])
            self.tt(pr(T2), pr(ABR), pr(LI), ALU.mult, [PT], [PT])
            self.tt(pr(T1), pr(T1), pr(T2), ALU.subtract, [PT], [PT])
            self.tt(pr(FIM), pr(T1), pr(DEN), ALU.mult, [PT], [PT])
            cosT = sb("cosT", [128, 16, T])
            sinT = sb("sinT", [128, 16, T])
            TB = pfx + "tab"
            with contextlib.ExitStack() as s2:
              if self.cut >= 2:
                  iot = self.sb(s2, pfx + "iot", [128, T])
                  self.P.op("pool", lambda e: e.iota(iot[:], [[1, T]], base=1, channel_multiplier=0,
                                                     allow_small_or_imprecise_dtypes=True),
                            writes=[self.tok(pfx + "iot")])
                  tmpA = self.sb(s2, pfx + "tmpA", [128, 16, T])
                  tmpN = self.sb(s2, pfx + "tmpN", [128, 16, T], I32)
                  ang = self.sb(s2, pfx + "ang", [128, 16, T])
                  self.tt(ang[:], prm[:, TH, :].unsqueeze(2).to_broadcast([128, 16, T]),
                          iot[:].unsqueeze(1).to_broadcast([128, 16, T]), ALU.mult, [PT, pfx + "iot"], [TB])
                  fl = lambda a: a[:].rearrange("p k t -> p (k t)")
                  reduce_angle(fl(ang), fl(tmpN), fl(tmpA), [TB])
                  self.act(fl(sinT), fl(ang), AF.Sin, [TB], [TB])
                  self.ts(fl(ang), fl(ang), math.pi / 2, None, ALU.add, None, [TB], [TB])
                  reduce_angle(fl(ang), fl(tmpN), fl(tmpA), [TB])
                  self.act(fl(cosT), fl(ang), AF.Sin, [TB], [TB])
            self.fence()
            BT = [sb("BTre", [128, 16, 128], BF16), sb("BTim", [128, 16, 128], BF16)]
            CT = [sb("CTre", [128, 16, 128], BF16), sb("CTnre", [128, 16, 128], BF16), sb("CTnim", [128, 16, 128], BF16)]
            with contextlib.ExitStack() as s2:
                bp = [self.sb(s2, pfx + f"bp{i}", [128, 16, 128]) for i in range(2)]
                bb = [self.sb(s2, pfx + f"bb{i}", [128, 16, 128]) for i in range(2)]
                cpad = [self.sb(s2, pfx + f"cp{i}", [128, 16, 128]) for i in range(2)]
                tmpB = self.sb(s2, pfx + "tmpB", [128, 16, 128])
                for i, nm in enumerate(("s5_b_re", "s5_b_im")):
                    self.memset(bp[i][:], 0.0, [pfx + f"bp{i}"])
                    for two in range(2):
                        for m in range(4):
                            dst = bp[i][two * 64:(two + 1) * 64, :, :].rearrange("p (j m) c -> p j m c", m=4)[
                                :, :, m, m * 32 + two * 16: m * 32 + two * 16 + 16]
                            src = dr[nm][l].rearrange("(j m two) p c -> two m p j c", m=4, two=2)[two, m]
                            self.dma("sync", dst, src, pfx + f"bp{i}", [], [pfx + f"bp{i}"])
                for i, nm in enumerate(("s5_c_re", "s5_c_im")):
                    self.memset(cpad[i][:], 0.0, [pfx + f"cp{i}"])
                    for two in range(2):
                        for m in range(4):
                            r0 = m * 32 + two * 16
                            dst = cpad[i][r0:r0 + 16, :, :].rearrange("p (j m) c -> p j m c", m=4)[
                                :, :, m, two * 64:(two + 1) * 64]
                            src = dr[nm][l].rearrange("(j m two) c p -> two m c j p", m=4, two=2)[two, m]
                            self.dma("sync", dst, src, pfx + f"cp{i}", [], [pfx + f"cp{i}"])
                fre_b = prm[:, FRE, :].unsqueeze(2).to_broadcast([128, 16, 128])
                fim_b = prm[:, FIM, :].unsqueeze(2).to_broadcast([128, 16, 128])
                B0, B1 = pfx + "bp0", pfx + "bp1"
                self.tt(bb[0][:], bp[0][:], fre_b, ALU.mult, [B0, PT], [pfx + "bb0"])
                self.tt(tmpB[:], bp[1][:], fim_b, ALU.mult, [B1, PT], [pfx + "tmpB"])
                self.tt(bb[0][:], bb[0][:], tmpB[:], ALU.subtract, [pfx + "bb0", pfx + "tmpB"], [pfx + "bb0"])
                self.tt(bb[1][:], bp[1][:], fre_b, ALU.mult, [B1, PT], [pfx + "bb1"])
                self.tt(tmpB[:], bp[0][:], fim_b, ALU.mult, [B0, PT], [pfx + "tmpB"])
                self.tt(bb[1][:], bb[1][:], tmpB[:], ALU.add, [pfx + "bb1", pfx + "tmpB"], [pfx + "bb1"])
                cnt = 0
                for i in range(2 if self.cut >= 3 else 0):
                    for k in range(16):
                        pb = PS[cnt % 2]; ptk = f"ps{cnt % 2}"
                        cnt += 1
                        self.tr(pb[:, 0:128], bb[i][:, k, :], self.ident[:], [pfx + f"bb{i}", "ident"], [ptk])
                        self.cp(BT[i][:, k, :], pb[:, 0:128], [ptk], [pfx + "BT"], eng="act" if k % 2 else "dve")
                for i in range(2 if self.cut >= 3 else 0):
                    for k in range(16):
                        pb = PS[cnt % 2]; ptk = f"ps{cnt % 2}"
                        cnt += 1
                        self.tr(pb[:, 0:128], cpad[i][:, k, :], self.ident[:], [pfx + f"cp{i}", "ident"], [ptk])
                        if i == 0:
                            self.cp(CT[0][:, k, :], pb[:, 0:128], [ptk], [pfx + "CT"], eng="act")
                            self.ts(CT[1][:, k, :], pb[:, 0:128], -1.0, None, ALU.mult, None, [ptk], [pfx + "CT"])
                        else:
                            self.ts(CT[2][:, k, :], pb[:, 0:128], -1.0, None, ALU.mult, None, [ptk], [pfx + "CT"])
            self.fence()
            sre = sb("sre", [128, 16]); sim = sb("sim", [128, 16])
            lre = sb("lre", [128, 16]); lim = sb("lim", [128, 16])
            ctmp = sb("ctmp", [128, 4, 16])
            self.memset(sre[:], 0.0, [pfx + "s5st"]); self.memset(sim[:], 0.0, [pfx + "s5st"])
            Sg = sb("Sg", [128, 2, 128])
            Sgbz = [[sb(f"Sgbz{r}{i}", [128, 2, 128], BF16) for i in range(2)] for r in range(2)]
            self.memset(Sg[:], 0.0, [pfx + "Sg"])
            for r in range(2):
                for i in range(2):
                    self.memset(Sgbz[r][i][:], 0.0, [pfx + f"Sgb{r}"])
            xs = [sb(f"xs{i}", [128, D]) for i in range(4)]
            scr = {"stat": sb("stat", [128, 4]), "junk": sb("junk", [128, D], BF16), "xn": sb("xn", [128, D])}
            hT = sb("hT", [128, 8, T], BF16)
            uT = sb("uT", [128, 4, T]); uTb = sb("uTb", [128, 4, T], BF16)
            qkT = sb("qkT", [128, 4, T])
            srT = sb("srT", [128, 4, T])
            glT = sb("glT", [16, T])
            vtm = sb("vtm", [128, 2, 512], BF16)
            ktm = sb("ktm", [128, 2, 256])
            kendz = [sb(f"kendz{i}", [128, 2, 256], BF16) for i in range(2)]
            qtT = sb("qtT", [128, 2, T], BF16)
            ktTz = [sb(f"ktTz{i}", [128, 2, T], BF16) for i in range(2)]
            for i in range(2):
                self.memset(kendz[i][:], 0.0, [pfx + "kend"])
                self.memset(ktTz[i][:], 0.0, [pfx + "ktT"])
            dec = sb("dec", [128, 2, 4])
            A = [[sb(f"A{q}{i}", [128, T]) for i in range(4)] for q in range(2)]
            vv = [[sb(f"vv{q}{i}", [128, T]) for i in range(2)] for q in range(2)]
            ss = [[sb(f"ss{q}{i}", [128, T]) for i in range(2)] for q in range(2)]
            pp = [[sb(f"pp{q}{i}", [128, T], BF16) for i in range(4)] for q in range(2)]
            yj = sb("yj", [128, T]); g1 = sb("g1", [128, T]); g2 = sb("g2", [128, T])
            yg = sb("yg", [128, 4, T]); ygb = sb("ygb", [128, 4, T], BF16)
            y2 = sb("y2", [128, 4, T]); sq = sb("sq", [128, 4, T], BF16)
            scTh = sb("scTh", [128, 4, 128], BF16)
            ltm = y2[:, 0:2, :]
            mixT = sb("mixT", [128, 8, T], BF16)
            o32 = yg
            p = lambda s: pfx + s

            for t in range(NT):
                t0 = t * T
                for b in range(2):
                    slot = (2 * t + b) % 4
                    xtok = p(f"xs{slot}")
                    self.dma("sync", xs[slot][:], xin[t0 + b * 128: t0 + (b + 1) * 128, :], xtok, [f"dram_{xin.tensor.name}"], [xtok])
                    self.norm_block(sk, pfx, xs[slot][:], xtok, gmix, p("gmix"),
                                    hT[:, :, b * 128:(b + 1) * 128], p("hT"), [PS[0], PS[1]], ["ps0", "ps1"], scr)
                def proj(n0, ncols, pout, ptok):
                    for k in range(8):
                        self.mm(pout, Win[:, k, n0:n0 + ncols], hT[:, k, :], k == 0, k == 7,
                                [p("hT")] + WIN, [ptok], sig=(k == 7))
                for j in range(12 if self.cut >= 4 else (int(round((self.cut - 3) * 100)) if self.cut > 3 else 0)):
                    n0 = [0, 128, 256, 384, 512, 640, 768, 896, 1536, 1664, 1792, 1920][j]
                    pout = PS[2 + j % 2][:, 0:256]
                    ptok = f"ps{2 + j % 2}"
                    proj(n0, 128, pout, ptok)
                    import os
                    dbgv = os.environ.get("DBGV", "")
                    if j < 4:
                        if dbgv != "A":
                            self.act(uT[:, j, :], pout, AF.Copy, [ptok], [p("uT")])
                        if dbgv not in ("A", "B"):
                            self.cp(uTb[:, j, :], uT[:, j, :], [p("uT")], [p("uTb")], eng="pool")
                    elif j < 8:
                        self.act(qkT[:, j - 4, :], pout, AF.Copy, [ptok], [p("qkT")])
                    else:
                        self.act(srT[:, j - 8, :], pout, AF.Silu, [ptok], [p("srT")])
                if self.cut >= 4.2:
                    proj(2048, 16, PS[6][0:16, 0:256], "ps6")
                    self.act(glT[:], PS[6][0:16, 0:256], AF.Copy, ["ps6"], [p("glT")])
                for b in range(2 if self.cut >= 4.3 else 0):
                    for k in range(8):
                        self.mm(PS[4][:, :], hT[:, k, b * 128:(b + 1) * 128], Win[:, k, 1024:1536], k == 0, k == 7,
                                [p("hT")] + WIN, ["ps4"], sig=(k == 7))
                    self.act(vtm[:, b, :], PS[4][:, :], AF.Copy, ["ps4"], [p("vtm")])
                    for k in range(8):
                        self.mm(PS[5][:, 0:256], hT[:, k, b * 128:(b + 1) * 128], Win[:, k, 768:1024], k == 0, k == 7,
                                [p("hT")] + WIN, ["ps5"], sig=(k == 7))
                    self.cp(ktm[:, b, :], PS[5][:, 0:256], ["ps5"], [p("ktm")])
                S5 = p("s5st")

                def st1(k):
                    j = k // 4
                    pb = PS[2 + (k % 2)]; ptk = f"ps{2 + (k % 2)}"
                    self.mm(pb[:, 0:T], BT[0][:, k, :], uTb[:, j, :], True, True, [p("BT"), p("uTb")], [ptk], sig=False)
                    self.mm(pb[:, T:2 * T], BT[1][:, k, :], uTb[:, j, :], True, True, [p("BT"), p("uTb")], [ptk])

                def st2(k):
                    q = k % 2
                    pb = PS[2 + q]; ptk = f"ps{2 + q}"
                    bre = pb[:, 0:T]; bim = pb[:, T:2 * T]
                    cr = cosT[:, k, :]; sr_ = sinT[:, k, :]
                    self.tt(A[q][0][:], bre, cr, ALU.mult, [ptk, TB], [p(f"A{q}0")])
                    self.tt(A[q][1][:], bim, sr_, ALU.mult, [ptk, TB], [p(f"A{q}1")])
                    self.tt(A[q][2][:], bim, cr, ALU.mult, [ptk, TB], [p(f"A{q}2")])
                    self.tt(A[q][3][:], bre, sr_, ALU.mult, [ptk, TB], [p(f"A{q}3")])
                    self.tt(vv[q][0][:], A[q][0][:], A[q][1][:], ALU.add, [p(f"A{q}0"), p(f"A{q}1")], [p(f"vv{q}0")], eng="pool")
                    self.tt(vv[q][1][:], A[q][2][:], A[q][3][:], ALU.subtract, [p(f"A{q}2"), p(f"A{q}3")], [p(f"vv{q}1")], eng="pool")

                def st3a(k):
                    q = k % 2
                    magb = prm[:, MAG, k:k + 1].to_broadcast([128, T])
                    self.P.op("dve", lambda e, o=ss[q][0][:], d1=vv[q][0][:], ini=sre[:, k:k + 1], mg=magb:
                              e.tensor_tensor_scan(out=o, data0=mg, data1=d1, initial=ini, op0=ALU.mult, op1=ALU.add),
                              reads=[self.tok(p(f"vv{q}0")), self.tok(S5), self.tok(PT)], writes=[self.tok(p(f"ss{q}0"))])
                    self.P.op("dve", lambda e, o=ss[q][1][:], d1=vv[q][1][:], ini=sim[:, k:k + 1], mg=magb:
                              e.tensor_tensor_scan(out=o, data0=mg, data1=d1, initial=ini, op0=ALU.mult, op1=ALU.add),
                              reads=[self.tok(p(f"vv{q}1")), self.tok(S5), self.tok(PT)], writes=[self.tok(p(f"ss{q}1"))])
                    self.act(lre[:, k:k + 1], ss[q][0][:, T - 1:T], AF.Copy, [p(f"ss{q}0")], [p("lst")])
                    self.act(lim[:, k:k + 1], ss[q][1][:, T - 1:T], AF.Copy, [p(f"ss{q}1")], [p("lst")])

                def st3b(k):
                    q = k % 2
                    j, m = k // 4, k % 4
                    cr = cosT[:, k, :]; sr_ = sinT[:, k, :]
                    self.tt(pp[q][0][:], ss[q][0][:], cr, ALU.mult, [p(f"ss{q}0"), TB], [p(f"pp{q}0")])
                    self.tt(pp[q][1][:], ss[q][1][:], sr_, ALU.mult, [p(f"ss{q}1"), TB], [p(f"pp{q}1")], eng="pool")
                    self.tt(pp[q][2][:], ss[q][0][:], sr_, ALU.mult, [p(f"ss{q}0"), TB], [p(f"pp{q}2")], eng="pool")
                    self.tt(pp[q][3][:], ss[q][1][:], cr, ALU.mult, [p(f"ss{q}1"), TB], [p(f"pp{q}3")],
                            eng=("pool" if k % 2 == 0 else "dve"))
                    ytk = f"ps{6 + j % 2}"
                    yps = PS[6 + j % 2][:, 0:T]
                    self.mm(yps, CT[0][:, k, :], pp[q][0][:], m == 0, False, [p("CT"), p(f"pp{q}0")], [ytk], sig=False)
                    self.mm(yps, CT[1][:, k, :], pp[q][1][:], False, False, [p("CT"), p(f"pp{q}1")], [ytk], sig=False)
                    self.mm(yps, CT[2][:, k, :], pp[q][2][:], False, False, [p("CT"), p(f"pp{q}2")], [ytk], sig=False)
                    self.mm(yps, CT[2][:, k, :], pp[q][3][:], False, m == 3, [p("CT"), p(f"pp{q}3")], [ytk])
                    if m == 3:
                        self.stt(yj[:], uT[:, j, :], dsk[:, j:j + 1], yps, ALU.mult, ALU.add, [p("uT"), p("dsk"), ytk], [p("yj")])
                        self.act(g1[:], yj[:], AF.Square, [p("yj")], [p("g1")])
                        self.ts(g1[:], g1[:], 0.044715, 1.0, ALU.mult, ALU.add, [p("g1")], [p("g1")])
                        self.tt(g1[:], g1[:], yj[:], ALU.mult, [p("g1"), p("yj")], [p("g1")])
                        self.act(g2[:], g1[:], AF.Sigmoid, [p("g1")], [p("g2")], scale=1.5957691216057308)
                        self.tt(yg[:, j, :], yj[:], g2[:], ALU.mult, [p("yj"), p("g2")], [p("yg")])
                        self.cp(ygb[:, j, :], yg[:, j, :], [p("yg")], [p("ygb")], eng="pool")

                if self.cut >= 5:
                    st1(0)
                    for k in range(16):
                        if k + 1 < 16:
                            st1(k + 1)
                        st2(k)
                        if k >= 1:
                            st3a(k - 1)
                            st3b(k - 1)
                    st3a(15)
                    st3b(15)
                if self.cut < 5:
                    self.emit_E(t, t0, xs, mixT, Wout, xout, p)
                    continue
                cT_ = cosT[:, :, T - 1]; sT_ = sinT[:, :, T - 1]
                self.tt(ctmp[:, 0, :], lre[:], cT_, ALU.mult, [p("lst"), TB], [p("ctmp")])
                self.tt(ctmp[:, 1, :], lim[:], sT_, ALU.mult, [p("lst"), TB], [p("ctmp")])
                self.tt(ctmp[:, 2, :], lre[:], sT_, ALU.mult, [p("lst"), TB], [p("ctmp")])
                self.tt(ctmp[:, 3, :], lim[:], cT_, ALU.mult, [p("lst"), TB], [p("ctmp")])
                self.tt(sre[:], ctmp[:, 0, :], ctmp[:, 1, :], ALU.subtract, [p("ctmp")], [S5])
                self.tt(sim[:], ctmp[:, 2, :], ctmp[:, 3, :], ALU.add, [p("ctmp")], [S5])
                Y2 = [p(f"y2{n}") for n in range(4)]
                SQ = [p(f"sq{n}") for n in range(4)]
                for n in range(4):
                    zp = PS[2 + n][:, 0:T]
                    for j in range(4):
                        self.mm(zp, Wglu[:, j, n * 128:(n + 1) * 128], ygb[:, j, :], j == 0, j == 3,
                                [p("Wglu"), p("ygb")], [f"ps{2 + n}"], sig=(j == 3))
                for n in range(4):
                    self.act(y2[:, n, :], PS[2 + n][:, 0:T], AF.Sigmoid, [f"ps{2 + n}", p("bglu")], [Y2[n]],
                             bias=bglu[:, n:n + 1])
                for n in range(4):
                    self.tt(y2[:, n, :], yg[:, n, :], y2[:, n, :], ALU.mult, [p("yg"), Y2[n]], [Y2[n]])
                for n in range(4):
                    self.act(sq[:, n, :], y2[:, n, :], AF.Square, [Y2[n]], [SQ[n]])
                msp = PS[6][:, 0:T]
                for n in range(4):
                    self.mm(msp, self.avg512[:], sq[:, n, :], n == 0, n == 3, ["avg512", SQ[n]], ["ps6"], sig=(n == 3))
                self.act(msp, msp, AF.Ln, ["ps6", "epsc"], ["ps6"], bias=self.epsc[:, 0:1])
                self.act(msp, msp, AF.Exp, ["ps6"], ["ps6"], scale=-0.5)
                for n in range(4):
                    self.stt(mixT[:, n, :], y2[:, n, :], gs5[:, n:n + 1], msp, ALU.mult, ALU.mult,
                             [Y2[n], p("gs5"), "ps6"], [p(f"mixT{n}")])
                if self.cut < 5.1:
                    self.emit_E(t, t0, xs, mixT, Wout, xout, p)
                    continue
                LT = [Y2[0], Y2[1]]
                for b in range(2):
                    bc = slice(b * 128, (b + 1) * 128)
                    zb = PS[7 - b]; ztk = f"ps{7 - b}"
                    zp = zb[:, 0:256]
                    self.mm(zp, glT[:, bc], Wa2[:], True, False, [p("glT"), p("Wa2")], [ztk], sig=False)
                    self.mm(zp, self.ones1[:], ba2[:], False, True, ["ones1", p("ba2")], [ztk])
                    self.act(ltm[:, b, :], zp, AF.Exp, [ztk], [LT[b]], scale=-1.0)
                    self.act(ltm[:, b, :], ltm[:, b, :], AF.Ln, [LT[b]], [LT[b]], bias=1.0)
                    for hp in range(2):
                        cb = PS[2 + 2 * b + hp]; ctk = f"ps{2 + 2 * b + hp}"
                        self.mm(cb[:, 0:128], ltm[:, b, hp * 128:(hp + 1) * 128], self.triM[:], True, True,
                                [LT[b], "triM"], [ctk])
                    rp = zb[:, 256:512]
                    self.mm(rp, self.triU[:], ltm[:, b, :], True, True, ["triU", LT[b]], [ztk])
                    for hp in range(2):
                        cb = PS[2 + 2 * b + hp]; ctk = f"ps{2 + 2 * b + hp}"
                        self.act(cb[:, 128:256], cb[:, 0:128], AF.Exp, [ctk], [ctk])
                        self.act(cb[:, 256:384], cb[:, 0:128], AF.Exp, [ctk], [ctk], scale=-1.0)
                    self.act(rp, rp, AF.Exp, [ztk], [ztk])
                    for hp in range(2):
                        cb = PS[2 + 2 * b + hp]; ctk = f"ps{2 + 2 * b + hp}"
                        Eq = cb[:, 128:256]; Ek = cb[:, 256:384]
                        self.ts(dec[:, hp, 2 * b:2 * b + 2], Eq.rearrange("p (c i) -> p c i", c=2)[:, :, 63], 1.0, None,
                                ALU.mult, None, [ctk], [p("dec")])
                        self.stt(qtT[:, hp, bc], qkT[:, hp, bc], 0.125, Eq, ALU.mult, ALU.mult,
                                 [p("qkT"), ctk], [p("qtT")])
                        for i in range(2):
                            hs = slice(i * 64, (i + 1) * 64)
                            self.tt(ktTz[i][hs, hp, bc], qkT[hs, 2 + hp, bc], Ek[hs, :], ALU.mult,
                                    [p("qkT"), ctk], [p("ktT")])
                    for i in range(2):
                        hs = slice(i * 64, (i + 1) * 64)
                        self.tt(kendz[i][hs, b, :], ktm[hs, b, :], rp[hs, :], ALU.mult,
                                [p("ktm"), ztk], [p("kend")])
                oP = [PS[6], PS[7]]
                if self.cut < 5.2:
                    self.emit_E(t, t0, xs, mixT, Wout, xout, p)
                    continue
                for b in range(2):
                    bc0 = b * 128
                    sbk = 2 + 2 * b; stk = f"ps{sbk}"
                    ubk = [3 + 2 * b, 2 + 2 * b]
                    for h in range(4):
                        hp = h // 2
                        self.mm(PS[sbk][:, h * 128:(h + 1) * 128], ktTz[h % 2][:, hp, bc0:bc0 + 128],
                                qtT[:, hp, bc0:bc0 + 128], True, True, [p("ktT"), p("qtT")], [stk], sig=(h == 3))
                    self.tt(scTh[:, :, :], PS[sbk][:, :].rearrange("p (h i) -> p h i", h=4),
                            self.maskC[:].unsqueeze(1).to_broadcast([128, 4, 128]), ALU.mult, [stk, "maskC"], [p("scTh")])
                    for c in range(2):
                        g = 2 * b + c
                        c0 = bc0 + c * 64
                        utk = f"ps{ubk[c]}"
                        for h in range(4):
                            hp = h // 2
                            self.mm(PS[ubk[c]][:, h * 128:(h + 1) * 128],
                                    kendz[c][:, b, hp * 128:(hp + 1) * 128],
                                    vtm[:, b, h * 128:(h + 1) * 128], True, True,
                                    [p("kend"), p("vtm")], [utk], sig=(h == 3))
                        rin = (g + 1) % 2
                        for h in range(4):
                            hp = h // 2
                            ob = oP[hp][:, (h % 2) * 256 + c0:(h % 2) * 256 + c0 + 64]
                            otk = f"ps{6 + hp}"
                            self.mm(ob, Sgbz[rin][h % 2][:, hp, :], qtT[:, hp, c0:c0 + 64], True, False,
                                    [p(f"Sgb{rin}"), p("qtT")], [otk], sig=False)
                            self.mm(ob, vtm[:, b, h * 128:(h + 1) * 128],
                                    scTh[:, h, c * 64:(c + 1) * 64], False, True,
                                    [p("vtm"), p("scTh")], [otk])
                        for h in range(4):
                            hp, po = h // 2, (h % 2) * 64
                            hs = slice(po, po + 64)
                            self.stt(Sg[hs, hp, :], Sg[hs, hp, :], dec[hs, hp, g:g + 1],
                                     PS[ubk[c]][hs, h * 128:(h + 1) * 128], ALU.mult, ALU.add,
                                     [p("Sg"), p("dec"), utk], [p("Sg")])
                        for i in range(2):
                            hs = slice(i * 64, (i + 1) * 64)
                            self.cp(Sgbz[g % 2][i][hs, :, :], Sg[hs, :, :], [p("Sg")], [p(f"Sgb{g % 2}")], eng="act")
                if self.cut < 5.5:
                    self.emit_E(t, t0, xs, mixT, Wout, xout, p)
                    continue
                for hp in range(2):
                    self.act(o32[:, 2 * hp:2 * hp + 2, :], oP[hp][:, :].rearrange("p (h t) -> p h t", h=2), AF.Copy,
                             [f"ps{6 + hp}"], [p("yg")])
                self.act(sq[:], o32[:], AF.Square, [p("yg")], SQ)
                for h in range(4):
                    self.mm(PS[2 + h][:, 0:256], self.avg128[:], sq[:, h, :], True, True, ["avg128", SQ[h]], [f"ps{2 + h}"])
                for h in range(4):
                    pb = PS[2 + h][:, 0:256]
                    self.act(pb, pb, AF.Ln, [f"ps{2 + h}", "epsc"], [f"ps{2 + h}"], bias=self.epsc[:, 0:1])
                for h in range(4):
                    pb = PS[2 + h][:, 0:256]
                    self.act(pb, pb, AF.Exp, [f"ps{2 + h}"], [f"ps{2 + h}"], scale=-0.5)
                for h in range(4):
                    pb = PS[2 + h][:, 0:256]
                    self.stt(pb, o32[:, h, :], ggla[:, h:h + 1], pb, ALU.mult, ALU.mult,
                             [p("yg"), p("ggla"), f"ps{2 + h}"], [f"ps{2 + h}"])
                for h in range(4):
                    pb = PS[2 + h][:, 0:256]
                    self.tt(mixT[:, 4 + h, :], pb, srT[:, h, :], ALU.mult, [f"ps{2 + h}", p("srT")], [p(f"mixT{4 + h}")])
                self.emit_E(t, t0, xs, mixT, Wout, xout, p)

    def emit_E(self, t, t0, xs, mixT, Wout, xout, p):
        PS = self.psum
        for b in range(2):
            slot = (2 * t + b) % 4
            xtok = p(f"xs{slot}")
            for half in range(2):
                pb = PS[half]
                for k in range(8):
                    self.mm(pb[:, :], mixT[:, k, b * 128:(b + 1) * 128], Wout[:, k, half * 512:(half + 1) * 512],
                            k == 0, k == 7, [p(f"mixT{k}"), p("Wout")], [f"ps{half}"], sig=(k == 7))
                self.tt(xs[slot][:, half * 512:(half + 1) * 512], pb[:, :], xs[slot][:, half * 512:(half + 1) * 512],
                        ALU.add, [f"ps{half}", xtok], [xtok])
            key = ("yout" if xout.tensor.name == "y" else p("st")) + str(b)
            self.dma("pool", xout[t0 + b * 128:t0 + (b + 1) * 128, :], xs[slot][:], key, [xtok],
                     [f"dram_{xout.tensor.name}"])

    def ffn(self, l, xin, xout, final):
        nc, dr, L = self.nc, self.dr, self.L
        pfx = f"f{l}_"
        p = lambda s: pfx + s
        PS = self.psum
        moe = (l == 1)
        ST = min(L, 2048)
        NST = L // ST
        NB = ST // 128
        NTL = ST // 256
        if moe:
            experts = list(range(NE)); FC = D_FFE // 128
        else:
            experts = [0]; FC = D_FF // 128
        slices = []
        for e in experts:
            c = 0
            while c < FC:
                n = min(4, FC - c)
                slices.append((e, c, n))
                c += n
        self.fence()
        with contextlib.ExitStack() as sk:
            sb = lambda name, shape, dt=F32: self.sb(sk, pfx + name, shape, dt)
            xacc = sb("xacc", [128, NB, D])
            hT = sb("hT", [128, 8, ST], BF16)
            gffn = sb("gffn", [128, 8]); self.load_colvec(gffn[:], dr["norm_ffn"][l], p("gffn"), p("gffn"))
            scr = {"stat": sb("stat", [128, 4]), "junk": sb("junk", [128, D], BF16), "xn": sb("xn", [128, D])}
            Wg = [sb(f"Wg{i}", [128, 8, 512], BF16) for i in range(2)]
            Wu = [sb(f"Wu{i}", [128, 8, 512], BF16) for i in range(2)]
            Wd = [sb(f"Wd{i}", [128, 4, D], BF16) for i in range(2)]
            sg = [sb(f"sg{i}", [128, 256]) for i in range(3)]
            aT = [sb(f"aT{i}", [128, 256], BF16) for i in range(6)]
            gates = sb("gates", [128, NB, NE])
            if moe:
                h32 = sb("h32", [128, 8, 128])
                Wr = sb("Wr", [128, 8, NE])
                self.dma("sync", Wr[:], dr["moe_w_router"][0].rearrange("(k q) e -> q k e", q=128), p("Wr"), [], [p("Wr")])
                lg = sb("lg", [128, 8]); mx = sb("mx", [128, 8]); rt = sb("rt", [128, 8]); gt = sb("gt", [128, 8])
            if final:
                gfin = sb("gfin", [128, D])
                self.dma("sync", gfin[:], dr["norm_final"].partition_broadcast(128), p("gfin"), [], [p("gfin")])
                yo = [sb(f"yo{i}", [128, D]) for i in range(2)]

            def wsrc(kind, e):
                if moe:
                    nm = {"g": "moe_w_gate", "u": "moe_w_up", "d": "moe_w_down"}[kind]
                    return dr[nm][0, e]
                nm = {"g": "ffn_w_gate", "u": "ffn_w_up", "d": "ffn_w_down"}[kind]
                return dr[nm][0]

            def load_slice(si, slot):
                e, c, n = slices[si]
                f0 = c * 128
                self.dma("pool", Wg[slot][:, :, 0:n * 128],
                         wsrc("g", e).rearrange("(k q) f -> q k f", q=128)[:, :, f0:f0 + n * 128],
                         p(f"Wg{slot}"), [], [p(f"Wg{slot}")])
                self.dma("pool", Wu[slot][:, :, 0:n * 128],
                         wsrc("u", e).rearrange("(k q) f -> q k f", q=128)[:, :, f0:f0 + n * 128],
                         p(f"Wu{slot}"), [], [p(f"Wu{slot}")])
                self.dma("pool", Wd[slot][:, 0:n, :],
                         wsrc("d", e)[f0:f0 + n * 128, :].rearrange("(c q) d -> q c d", q=128),
                         p(f"Wd{slot}"), [], [p(f"Wd{slot}")])

            for s in range(NST):
                r0 = s * ST
                load_slice(0, 0)
                for b in range(NB):
                    xtok = p(f"xacc{b}")
                    self.dma("sync", xacc[:, b, :], xin[r0 + b * 128:r0 + (b + 1) * 128, :], p(f"xl{b % 4}"),
                             [f"dram_{xin.tensor.name}"], [xtok])
                    self.norm_block(sk, pfx, xacc[:, b, :], xtok, gffn, p("gffn"),
                                    hT[:, :, b * 128:(b + 1) * 128], p("hT"), [PS[6], PS[7]], ["ps6", "ps7"], scr,
                                    h32_dst=(h32 if moe else None))
                    if moe:
                        lp = PS[5][:, 0:NE]
                        for k in range(8):
                            self.mm(lp, h32[:, k, :], Wr[:, k, :], k == 0, k == 7, [p("hT32"), p("Wr")], ["ps5"], sig=(k == 7))
                        G = p("gt")
                        self.cp(lg[:], lp, ["ps5"], [G])
                        self.P.op("dve", lambda e: e.max(out=mx[:], in_=lg[:]), reads=[self.tok(G)], writes=[self.tok(G)])
                        self.tt(rt[:, 0:1], mx[:, 1:2], mx[:, 0:1], ALU.subtract, [G], [G])
                        self.act(rt[:, 1:2], rt[:, 0:1], AF.Exp, [G], [G])
                        self.ts(rt[:, 2:3], rt[:, 1:2], 1.0, None, ALU.add, None, [G], [G])
                        self.P.op("dve", lambda e: e.reciprocal(out=rt[:, 3:4], in_=rt[:, 2:3]), reads=[self.tok(G)], writes=[self.tok(G)])
                        self.tt(rt[:, 4:5], rt[:, 1:2], rt[:, 3:4], ALU.mult, [G], [G])
                        self.ts(gt[:], lg[:], mx[:, 0:1], rt[:, 3:4], ALU.is_equal, ALU.mult, [G], [G])
                        self.ts(lg[:], lg[:], mx[:, 1:2], rt[:, 4:5], ALU.is_equal, ALU.mult, [G], [G])
                        self.tt(gates[:, b, :], gt[:], lg[:], ALU.add, [G], [p("gates")])
                units = [(si, tl, cc) for si, (e_, c_, n_) in enumerate(slices) for tl in range(NTL) for cc in range(n_)]
                NU = len(units)
                loaded = {0}

                def emit_gu(u):
                    si, tl, cc = units[u]
                    e, c, n = slices[si]
                    slot = si % 2
                    WT = [p(f"Wg{slot}"), p(f"Wu{slot}"), p(f"Wd{slot}")]
                    tc = slice(tl * 256, (tl + 1) * 256)
                    gb = PS[4 + u % 4]
                    gtk = f"ps{4 + u % 4}"
                    gp = gb[:, 0:256]; up = gb[:, 256:512]
                    for k in range(8):
                        self.mm(gp, Wg[slot][:, k, cc * 128:(cc + 1) * 128], hT[:, k, tc], k == 0, k == 7,
                                [p("hT"), WT[0]], [gtk], sig=False)
                    for k in range(8):
                        self.mm(up, Wu[slot][:, k, cc * 128:(cc + 1) * 128], hT[:, k, tc], k == 0, k == 7,
                                [p("hT"), WT[1]], [gtk], sig=(k == 7))
                    sgi = sg[u % 3]
                    self.act(sgi[:], gp, AF.Silu, [gtk], [p(f"sg{u % 3}")])
                    self.tt(aT[u % 6][:], sgi[:], up, ALU.mult, [p(f"sg{u % 3}"), gtk], [p(f"aT{u % 6}")])

                def emit_down(u):
                    si, tl, cc = units[u]
                    e, c, n = slices[si]
                    slot = si % 2
                    if si + 1 < len(slices) and (si + 1) not in loaded:
                        loaded.add(si + 1)
                        load_slice(si + 1, 1 - slot)
                    for b2 in range(2):
                        for half in range(2):
                            ai = b2 * 2 + half
                            self.mm(PS[ai][:, :], aT[u % 6][:, b2 * 128:(b2 + 1) * 128],
                                    Wd[slot][:, cc, half * 512:(half + 1) * 512], cc == 0, cc == n - 1,
                                    [p(f"aT{u % 6}"), p(f"Wd{slot}")], self.pst(ai), sig=(cc == n - 1))
                    if cc == n - 1:
                        for b2 in range(2):
                            b = tl * 2 + b2
                            xtok = p(f"xacc{b}")
                            for half in range(2):
                                ai = b2 * 2 + half
                                dst = xacc[:, b, half * 512:(half + 1) * 512]
                                if moe:
                                    self.stt(dst, PS[ai][:, :], gates[:, b, e:e + 1], dst, ALU.mult, ALU.add,
                                             self.pst(ai) + [p("gates"), xtok], [xtok])
                                else:
                                    self.tt(dst, PS[ai][:, :], dst, ALU.add, self.pst(ai) + [xtok], [xtok])

                for u in range(min(3, NU)):
                    emit_gu(u)
                for u in range(NU):
                    if u + 3 < NU:
                        emit_gu(u + 3)
                    emit_down(u)
                for b in range(NB):
                    xtok = p(f"xacc{b}")
                    rows = slice(r0 + b * 128, r0 + (b + 1) * 128)
                    if final:
                        o = b % 2
                        stt_ = scr["stat"]
                        self.act(scr["junk"][:], xacc[:, b, :], AF.Square, [xtok], [p("junk"), p("stat")], accum=stt_[:, 0:1])
                        self.rsqrt_(stt_[:, 1:2], stt_[:, 0:1], stt_[:, 2:3], [p("stat")], [p("stat")], mul=1.0 / D)
                        self.stt(yo[o][:], xacc[:, b, :], stt_[:, 1:2], gfin[:], ALU.mult, ALU.mult,
                                 [xtok, p("stat"), p("gfin")], [p(f"yo{o}")])
                        self.dma("sync", xout[rows, :], yo[o][:], f"yout{o}", [p(f"yo{o}")], [f"dram_{xout.tensor.name}"])
                    else:
                        key = ("yout" if xout.tensor.name == "y" else p("st")) + str(b % 2)
                        self.dma("sync", xout[rows, :], xacc[:, b, :], key, [xtok], [f"dram_{xout.tensor.name}"])


_CACHE = {}


def get_nc(L, stop_after=None):
    key = (L, stop_after)
    if key not in _CACHE:
        _CACHE[key] = K(L, stop_after).build()
    return _CACHE[key]


NAMES = ["norm_mix", "w_in", "s5_lambda_re", "s5_lambda_im", "s5_log_dt", "s5_b_re", "s5_b_im", "s5_c_re", "s5_c_im",
         "s5_d", "s5_w_glu", "s5_b_glu", "s5_out_norm", "gla_w_a2", "gla_b_a2", "gla_out_norm", "w_out", "norm_ffn",
         "ffn_w_gate", "ffn_w_up", "ffn_w_down", "moe_w_router", "moe_w_gate", "moe_w_up", "moe_w_down", "norm_final"]


def kernel(**inputs):
    x = np.ascontiguousarray(np.asarray(inputs["x"], dtype=np.float32))
    B, L, _ = x.shape
    nc = get_nc(L)
    shared = {n: np.ascontiguousarray(np.asarray(inputs[n], dtype=np.float32)) for n in NAMES}
    in_maps = []
    for b in range(B):
        m = dict(shared)
        m["x"] = x[b]
        in_maps.append(m)
    res = run_bass_kernel_spmd(nc, in_maps, core_ids=list(range(B)))
    return np.stack([np.asarray(r["y"], dtype=np.float32) for r in res.results], axis=0)
```
